# Optimizing a Trainium2 kernel written in Bass

```python
import jax, jax.numpy as jnp
from jax import lax
import numpy as np

D_MODEL = 1024
BATCH = 16
SEQ = 2048
DEPTH = 1

HEAD_DIM = 64
GQA_HEADS = 8
GQA_KV_HEADS = 2
GQA_GROUP = GQA_HEADS // GQA_KV_HEADS
MLA_HEADS = 8
MLA_NOPE_DIM = 64
MLA_ROPE_DIM = 32
MLA_V_DIM = 64
MLA_Q_RANK = 256
MLA_KV_RANK = 256
MIX_WIDTH = GQA_HEADS * HEAD_DIM + MLA_HEADS * MLA_V_DIM
SZ_GQA_Q = GQA_HEADS * HEAD_DIM
SZ_GQA_K = GQA_KV_HEADS * HEAD_DIM
SZ_GQA_V = GQA_KV_HEADS * HEAD_DIM
SZ_MLA_CQ = MLA_Q_RANK
SZ_MLA_CKV = MLA_KV_RANK
SZ_MLA_KR = MLA_ROPE_DIM
IN_WIDTH = SZ_GQA_Q + SZ_GQA_K + SZ_GQA_V + SZ_MLA_CQ + SZ_MLA_CKV + SZ_MLA_KR
SPLIT_1 = SZ_GQA_Q
SPLIT_2 = SPLIT_1 + SZ_GQA_K
SPLIT_3 = SPLIT_2 + SZ_GQA_V
SPLIT_4 = SPLIT_3 + SZ_MLA_CQ
SPLIT_5 = SPLIT_4 + SZ_MLA_CKV
N_EXPERTS = 16
CAPACITY_FACTOR = 2
EXPERT_FF = 2816
PLE_DIM = 256
GRID_W = 64
Q_BLOCK = 128
ROPE_BASE = 10000.0
LN_EPS = 1e-5
RMS_EPS = 1e-6
DN_ALPHA = (2 * DEPTH) ** 0.25
DN_BETA = (8 * DEPTH) ** -0.25

kernel_name = 'hybrid_gqa_mla_ec_moe_encoder'


def layer_norm(x, g, b):
    xf = x.astype(jnp.float32)
    mu = jnp.mean(xf, axis=-1, keepdims=True)
    xc = xf - mu
    var = jnp.mean(xc * xc, axis=-1, keepdims=True)
    return (xc * lax.rsqrt(var + LN_EPS) * g.astype(jnp.float32) + b.astype(jnp.float32)).astype(x.dtype)


def rms_norm(x, g):
    xf = x.astype(jnp.float32)
    ms = jnp.mean(xf * xf, axis=-1, keepdims=True)
    return (xf * lax.rsqrt(ms + RMS_EPS) * g.astype(jnp.float32)).astype(x.dtype)


def rope_1d(x, pos):
    d = x.shape[-1]
    half = d // 2
    inv_freq = ROPE_BASE ** (-jnp.arange(half, dtype=jnp.float32) * 2.0 / d)
    ang = pos[:, None] * inv_freq[None, :]
    cos = jnp.cos(ang)[:, None, :]
    sin = jnp.sin(ang)[:, None, :]
    xf = x.astype(jnp.float32)
    x1, x2 = xf[..., :half], xf[..., half:]
    return jnp.concatenate([x1 * cos - x2 * sin, x2 * cos + x1 * sin], axis=-1).astype(x.dtype)


def axial_rope(x, row, col):
    h = x.shape[-1] // 2
    return jnp.concatenate([rope_1d(x[..., :h], row), rope_1d(x[..., h:], col)], axis=-1)


def block_attention(q, k, v, scale):
    B, S, KH, G, Dk = q.shape
    nb = S // Q_BLOCK
    qb = q.reshape(B, nb, Q_BLOCK, KH, G, Dk).transpose(1, 0, 2, 3, 4, 5)

    def one_block(qblk):
        s = jnp.einsum('bqhgd,bkhd->bhgqk', qblk, k, preferred_element_type=jnp.float32) * scale
        w = jax.nn.softmax(s, axis=-1).astype(v.dtype)
        return jnp.einsum('bhgqk,bkhd->bqhgd', w, v)

    o = lax.map(one_block, qb)
    return o.transpose(1, 0, 2, 3, 4, 5).reshape(B, S, KH, G, v.shape[-1])


def setup_inputs(seed: int = 0) -> dict:
    key = jax.random.key(seed)
    ks = jax.random.split(key, 24)
    f32 = jnp.float32

    def nrm(k, shape, scale):
        return jax.random.normal(k, shape, f32) * scale

    def gain(k, shape):
        return 1.0 + 0.02 * jax.random.normal(k, shape, f32)

    L = DEPTH
    return {
        'x': jax.random.normal(ks[0], (BATCH, SEQ, D_MODEL), f32),
        'p': jax.random.normal(ks[1], (DEPTH, BATCH, SEQ, PLE_DIM), f32),
        'w_in': nrm(ks[2], (L, D_MODEL, IN_WIDTH), D_MODEL ** -0.5),
        'q_norm': gain(ks[3], (L, HEAD_DIM)),
        'k_norm': gain(ks[4], (L, HEAD_DIM)),
        'cq_norm': gain(ks[5], (L, MLA_Q_RANK)),
        'ckv_norm': gain(ks[6], (L, MLA_KV_RANK)),
        'w_uq': nrm(ks[7], (L, MLA_Q_RANK, MLA_HEADS * (MLA_NOPE_DIM + MLA_ROPE_DIM)), MLA_Q_RANK ** -0.5),
        'w_ukv': nrm(ks[8], (L, MLA_KV_RANK, MLA_HEADS * (MLA_NOPE_DIM + MLA_V_DIM)), MLA_KV_RANK ** -0.5),
        'w_out': nrm(ks[9], (L, MIX_WIDTH, D_MODEL), DN_BETA * MIX_WIDTH ** -0.5),
        'ln_attn_g': gain(ks[10], (L, D_MODEL)),
        'ln_attn_b': nrm(ks[11], (L, D_MODEL), 0.02),
        'w_router': nrm(ks[12], (L, D_MODEL, N_EXPERTS), D_MODEL ** -0.5),
        'w_gate': nrm(ks[13], (L, N_EXPERTS, D_MODEL, EXPERT_FF), D_MODEL ** -0.5),
        'w_up': nrm(ks[14], (L, N_EXPERTS, D_MODEL, EXPERT_FF), D_MODEL ** -0.5),
        'w_down': nrm(ks[15], (L, N_EXPERTS, EXPERT_FF, D_MODEL), DN_BETA * EXPERT_FF ** -0.5),
        'ln_ffn_g': gain(ks[16], (L, D_MODEL)),
        'ln_ffn_b': nrm(ks[17], (L, D_MODEL), 0.02),
        'w_ple_proj': nrm(ks[18], (L, PLE_DIM, D_MODEL), DN_BETA * PLE_DIM ** -0.5),
        'w_ple_gate': nrm(ks[19], (L, D_MODEL, D_MODEL), D_MODEL ** -0.5),
        'ln_ple_g': gain(ks[20], (L, D_MODEL)),
        'ln_ple_b': nrm(ks[21], (L, D_MODEL), 0.02),
    }


def reference(x, p, w_in, q_norm, k_norm, cq_norm, ckv_norm, w_uq, w_ukv, w_out,
              ln_attn_g, ln_attn_b, w_router, w_gate, w_up, w_down, ln_ffn_g, ln_ffn_b,
              w_ple_proj, w_ple_gate, ln_ple_g, ln_ple_b):
    B, S, _ = x.shape
    rows = S // GRID_W
    row = jnp.repeat(jnp.arange(rows, dtype=jnp.float32), GRID_W)
    col = jnp.tile(jnp.arange(GRID_W, dtype=jnp.float32), rows)
    cap = CAPACITY_FACTOR * S // N_EXPERTS
    bidx = jnp.arange(B)[:, None, None]

    for i in range(DEPTH):
        proj = jnp.einsum('bsd,dn->bsn', x, w_in[i])
        q_g, k_g, v_g, c_q, c_kv, k_r = jnp.split(
            proj, [SPLIT_1, SPLIT_2, SPLIT_3, SPLIT_4, SPLIT_5], axis=-1)

        q = axial_rope(rms_norm(q_g.reshape(B, S, GQA_HEADS, HEAD_DIM), q_norm[i]), row, col)
        k = axial_rope(rms_norm(k_g.reshape(B, S, GQA_KV_HEADS, HEAD_DIM), k_norm[i]), row, col)
        v = v_g.reshape(B, S, GQA_KV_HEADS, HEAD_DIM)
        q = q.reshape(B, S, GQA_KV_HEADS, GQA_GROUP, HEAD_DIM)
        o_gqa = block_attention(q, k, v, HEAD_DIM ** -0.5).reshape(B, S, GQA_HEADS * HEAD_DIM)

        qm = jnp.einsum('bsr,rn->bsn', rms_norm(c_q, cq_norm[i]), w_uq[i])
        qm = qm.reshape(B, S, MLA_HEADS, MLA_NOPE_DIM + MLA_ROPE_DIM)
        q_pe = axial_rope(qm[..., MLA_NOPE_DIM:], row, col)
        q_m = jnp.concatenate([qm[..., :MLA_NOPE_DIM], q_pe], axis=-1)[:, :, :, None, :]
        kv = jnp.einsum('bsr,rn->bsn', rms_norm(c_kv, ckv_norm[i]), w_ukv[i])
        kv = kv.reshape(B, S, MLA_HEADS, MLA_NOPE_DIM + MLA_V_DIM)
        k_pe = axial_rope(k_r[:, :, None, :], row, col)
        k_m = jnp.concatenate(
            [kv[..., :MLA_NOPE_DIM], jnp.broadcast_to(k_pe, (B, S, MLA_HEADS, MLA_ROPE_DIM))], axis=-1)
        v_m = kv[..., MLA_NOPE_DIM:]
        o_mla = block_attention(q_m, k_m, v_m, (MLA_NOPE_DIM + MLA_ROPE_DIM) ** -0.5)
        o_mla = o_mla.reshape(B, S, MLA_HEADS * MLA_V_DIM)

        mix = jnp.einsum('bsm,md->bsd', jnp.concatenate([o_gqa, o_mla], axis=-1), w_out[i])
        x = layer_norm(DN_ALPHA * x + mix, ln_attn_g[i], ln_attn_b[i])

        logits = jnp.einsum('bsd,de->bse', x, w_router[i], preferred_element_type=jnp.float32)
        aff = jax.nn.softmax(logits, axis=-1).transpose(0, 2, 1)
        g_val, t_idx = lax.top_k(aff, cap)
        xg = x[bidx, t_idx]
        h = jax.nn.silu(jnp.einsum('becd,edf->becf', xg, w_gate[i])) * \
            jnp.einsum('becd,edf->becf', xg, w_up[i])
        y = jnp.einsum('becf,efd->becd', h, w_down[i]) * g_val[..., None].astype(x.dtype)
        moe = jnp.zeros_like(x).at[bidx, t_idx].add(y)
        x = layer_norm(DN_ALPHA * x + moe, ln_ffn_g[i], ln_ffn_b[i])

        e = jnp.einsum('bsp,pd->bsd', p[i], w_ple_proj[i])
        gate = jax.nn.sigmoid(jnp.einsum('bsd,de->bse', x, w_ple_gate[i]))
        x = layer_norm(DN_ALPHA * x + gate * e, ln_ple_g[i], ln_ple_b[i])

    return x
```

```python
import contextlib
import numpy as np
import ml_dtypes
import concourse.bass as bass
import concourse.mybir as mybir
from concourse.bass_utils import run_bass_kernel_spmd

F32 = mybir.dt.float32
BF16 = mybir.dt.bfloat16
U32 = mybir.dt.uint32
AF = mybir.ActivationFunctionType
ALU = mybir.AluOpType
AX = mybir.AxisListType

NCORES = 8
SPC = 2
S = 2048
D = 1024
NT = S // 128
E = 16
CAP = 256
FF = 2816
NFC = FF // 128
PLE = 256
ALPHA = float(2.0 ** 0.25)
LN_EPS = 1e-5
RMS_EPS = 1e-6
NSLOT = 8
DEBUG = False


class Buf:
    __slots__ = ("name", "w", "r", "psum")

    def __init__(self, name="", psum=False):
        self.name = name
        self.w = {}
        self.r = {}
        self.psum = psum


class T:
    _ctr = [0]

    def __init__(self, nc, stack, name, shape, dtype):
        T._ctr[0] += 1
        self.t = stack.enter_context(nc.sbuf_tensor("%s_t%d" % (name, T._ctr[0]), list(shape), dtype))
        self.b = Buf(name)


class Sched:
    ENG = ("pe", "dve", "act", "pool", "sp")

    def __init__(self, nc, stack):
        self.nc = nc
        self.stack = stack
        self.esem = {e: stack.enter_context(nc.semaphore("sem_" + e)) for e in self.ENG}
        self.ecount = {e: 0 for e in self.ENG}
        self.seen = {e: {} for e in self.ENG}
        self.lists = {e: [] for e in self.ENG}
        self.dsems = {}
        self.nops = 0

    def _need(self, e, waits, ev, same_ok):
        sem, val, src = ev
        if same_ok and src == e:
            return
        k = id(sem)
        if self.seen[e].get(k, 0) >= val:
            return
        if k not in waits or waits[k][1] < val:
            waits[k] = (sem, val)

    def _deps(self, e, reads, writes, accw, is_dma):
        waits = {}
        for b in reads:
            for ev in b.w.values():
                self._need(e, waits, ev, (e == "pe") and not is_dma)
            if b.psum:
                for ev in b.r.values():
                    self._need(e, waits, ev, True)
        for b in writes:
            for ev in b.w.values():
                self._need(e, waits, ev, not is_dma)
            for ev in b.r.values():
                self._need(e, waits, ev, not is_dma)
        for b in accw:
            for ev in b.r.values():
                self._need(e, waits, ev, not is_dma)
        return waits

    @staticmethod
    def _put(d, ev):
        k = id(ev[0])
        if k not in d or d[k][1] < ev[1]:
            d[k] = ev

    def _commit(self, e, waits, ev, reads, writes, accw):
        for k, (sem, val) in waits.items():
            self.seen[e][k] = val
        for b in reads:
            self._put(b.r, ev)
        for b in writes:
            b.w = {id(ev[0]): ev}
            b.r = {}
        for b in accw:
            self._put(b.w, ev)

    def op(self, e, fn, reads=(), writes=(), accw=()):
        waits = self._deps(e, reads, writes, accw, False)
        self.ecount[e] += 1
        ev = (self.esem[e], self.ecount[e], e)
        self.lists[e].append((list(waits.values()), fn, self.esem[e], 1))
        self._commit(e, waits, ev, reads, writes, accw)
        self.nops += 1

    def dma(self, q, fn, key, reads=(), writes=(), accw=()):
        waits = self._deps(q, reads, writes, accw, True)
        if key not in self.dsems:
            self.dsems[key] = [self.stack.enter_context(self.nc.semaphore("d%d" % len(self.dsems))), 0]
        ent = self.dsems[key]
        if ent[1] > 0:
            self._need(q, waits, (ent[0], ent[1], "dma"), False)
        ent[1] += 16
        ev = (ent[0], ent[1], "dma")
        self.lists[q].append((list(waits.values()), fn, ent[0], 16))
        self._commit(q, waits, ev, reads, writes, accw)
        self.nops += 1

    def barrier(self):
        for e in self.ENG:
            waits = {}
            for f in self.ENG:
                if f != e and self.ecount[f] > 0:
                    self._need(e, waits, (self.esem[f], self.ecount[f], f), False)
            for ent in self.dsems.values():
                if ent[1] > 0:
                    self._need(e, waits, (ent[0], ent[1], "dma"), False)
            if waits:
                self.lists[e].append((list(waits.values()), None, None, 0))
                for k, (sem, val) in waits.items():
                    self.seen[e][k] = val

    def flush(self):
        self.barrier()
        nc = self.nc
        lists = self.lists
        self.lists = {e: [] for e in self.ENG}

        def body_for(e):
            ops = lists[e]

            def body(eng):
                for waits, fn, sem, inc in ops:
                    for (ws, wv) in waits:
                        eng.wait_ge(ws, wv)
                    if fn is not None:
                        ins = fn(eng)
                        ins.then_inc(sem, inc)
            return body

        with nc.Block() as blk:
            blk.tensor(body_for("pe"))
            blk.vector(body_for("dve"))
            blk.scalar(body_for("act"))
            blk.gpsimd(body_for("pool"))
            blk.sync(body_for("sp"))


class StopBuild(Exception):
    pass


def build_program(stop=None):
    nc = bass.Bass("TRN2", target_bir_lowering=False)
    dumps = {}

    def din(name, shape, dtype):
        return nc.dram_tensor(name, list(shape), dtype, kind="ExternalInput").ap()

    x_d = din("x", [SPC, S, D], F32)
    p_d = din("p", [SPC, S, PLE], F32)
    w_in_d = din("w_in_p", [D, 1312], F32)
    w_rot_d = din("w_in_rot", [D, 672], F32)
    w_uq_d = din("w_uq_cat", [256, 1024], F32)
    w_ukv_d = din("w_ukv", [256, 1024], F32)
    w_out_d = din("w_out_p", [D, D], F32)
    w_r_d = din("w_router", [D, E], F32)
    if stop in (None, "B", "C"):
        wg_d = din("w_gate", [E, D, FF], F32)
        wu_d = din("w_up", [E, D, FF], F32)
        wd_d = din("w_down", [E, FF, D], F32)
    wpp_d = din("w_ple_proj", [PLE, D], F32)
    wpg_d = din("w_ple_gate", [D, D], F32)
    lnp_d = din("lnp", [6, 128, D], F32)
    vecs_d = din("vecs", [128, 8], F32)
    tabs_d = din("tabs", [4, 128, S], F32)
    identf_d = din("ident_f", [128, 128], F32)
    cb_d = din("cb", [128, 3, 128], BF16)
    out_d = nc.dram_tensor("out", [SPC, S, D], F32, kind="ExternalOutput").ap()
    skind = "ExternalOutput" if (DEBUG or stop is not None) else "Internal"
    x1b_d = nc.dram_tensor("x1b_scr", [SPC * S, D], BF16, kind=skind).ap()
    acc_d = nc.dram_tensor("acc_scr", [SPC * S, D], F32, kind=skind).ap()
    wdb_d = nc.dram_tensor("wdb_scr", [E, FF, D], BF16, kind="Internal").ap()
    PRECAST = stop in (None, "B", "C")

    try:
      with contextlib.ExitStack() as top:
        sch = Sched(nc, top)

        def dump(name, t, shape=None):
            ap = t.t[:]
            d = nc.dram_tensor("dbg_" + name, list(ap.shape), ap.dtype, kind="ExternalOutput").ap()
            dumps[name] = d
            sch.dma("sp", lambda q: q.dma_start(out=d, in_=ap), "dump_" + name, reads=[t.b] + (shape or []))

        def stop_here(tag):
            if stop == tag:
                sch.flush()
                raise StopBuild()
        ps = top.enter_context(nc.psum_tensor("ps", [128, 8, 512], F32))
        PB = [Buf("pb%d" % i, psum=True) for i in range(8)]
        bank_ctr = [0]
        pair_ctr = [0]

        def nbank():
            bank_ctr[0] = (bank_ctr[0] + 1) % 8
            return bank_ctr[0]

        def npair():
            pair_ctr[0] = (pair_ctr[0] + 1) % 4
            return 2 * pair_ctr[0]

        def ps2(b0):
            return ps[:, b0:b0 + 2, :].rearrange("p a b -> p (a b)")

        identf = T(nc, top, "identf", [128, 128], F32)
        cb = T(nc, top, "cb", [128, 3, 128], BF16)
        vecs = T(nc, top, "vecs", [128, 8], F32)
        affT = T(nc, top, "affT", [48, S], F32)
        idxT = T(nc, top, "idxT", [128, 2, 48], U32)
        gT = T(nc, top, "gT", [128, 2, 48], F32)
        x1b_buf = Buf("x1b_dram")
        acc_bufs = [Buf("acc_dram%d" % b) for b in range(SPC)]
        wdb_bufs = [Buf("wdb%d" % e) for e in range(E)]

        sch.dma("sp", lambda q: q.dma_start(out=identf.t[:], in_=identf_d), "c0", writes=[identf.b])
        sch.dma("sp", lambda q: q.dma_start(out=cb.t[:], in_=cb_d), "c1", writes=[cb.b])
        sch.dma("sp", lambda q: q.dma_start(out=vecs.t[:], in_=vecs_d), "c2", writes=[vecs.b])
        sch.op("pool", lambda g: g.memset(affT.t[:], 0.0), writes=[affT.b])
        epsr = T(nc, top, "epsr", [128, 4], F32)
        sch.op("pool", lambda g: g.memset(epsr.t[:, 0:1], RMS_EPS), writes=[epsr.b])
        sch.op("pool", lambda g: g.memset(epsr.t[:, 1:2], LN_EPS), accw=[epsr.b])
        sch.op("pool", lambda g: g.memset(epsr.t[:, 2:3], LN_EPS / (ALPHA * ALPHA)), accw=[epsr.b])
        identb = cb.t[:, 0, :]
        blockones = cb.t[:, 1, :]
        ones256 = cb.t[:, 2, :]

        def cast_load(dst_t, dst_ap, src_ap, key):
            sch.dma("pool", lambda q: q.dma_start(out=dst_ap, in_=src_ap), key, writes=[dst_t.b])


        def pipeline(n, stages):
            ns = len(stages)
            for t in range(n + ns - 1):
                for s in reversed(range(ns)):
                    i = t - s
                    if 0 <= i < n:
                        stages[s](i)

        def ln_stats(src_t, stt):
            def f(v):
                v.bn_stats(out=stt.t[:, 0:6], in_=src_t.t[:, 0:512])
                return v.bn_stats(out=stt.t[:, 6:12], in_=src_t.t[:, 512:1024])
            sch.op("dve", f, reads=[src_t.b], writes=[stt.b])
            sch.op("dve", lambda v: v.bn_aggr(out=stt.t[:, 12:14], in_=stt.t[:, 0:12]), reads=[stt.b], accw=[stt.b])
            sch.op("act", lambda a: a.activation(out=stt.t[:, 14:15], in_=stt.t[:, 13:14], func=AF.Sqrt, bias=epsr.t[:, 1:2], scale=1.0),
                   reads=[stt.b, epsr.b], accw=[stt.b])
            sch.op("dve", lambda v: v.reciprocal(out=stt.t[:, 14:15], in_=stt.t[:, 14:15]), reads=[stt.b], accw=[stt.b])
            sch.op("dve", lambda v: v.scalar_tensor_tensor(out=stt.t[:, 15:16], in0=stt.t[:, 12:13], scalar=-1.0, in1=stt.t[:, 14:15],
                                                           op0=ALU.mult, op1=ALU.mult), reads=[stt.b], accw=[stt.b])

        def ln_apply(src_t, stt, g_t, b_t, tmp_t, dst_t, g_eng="pool", b_eng="pool"):
            sch.op("act", lambda a: a.activation(out=tmp_t.t[:], in_=src_t.t[:], func=AF.Identity, bias=stt.t[:, 15:16], scale=stt.t[:, 14:15]),
                   reads=[src_t.b, stt.b], writes=[tmp_t.b])
            sch.op(g_eng, lambda g: g.tensor_tensor(out=tmp_t.t[:], in0=tmp_t.t[:], in1=g_t.t[:], op=ALU.mult),
                   reads=[tmp_t.b, g_t.b], writes=[tmp_t.b])
            sch.op(b_eng, lambda g: g.tensor_tensor(out=dst_t.t[:], in0=tmp_t.t[:], in1=b_t.t[:], op=ALU.add),
                   reads=[tmp_t.b, b_t.b], writes=[dst_t.b])

        for b in range(SPC):
            with contextlib.ExitStack() as sa:
                big = T(nc, sa, "big", [128, 8, S], BF16)
                bigb = [[Buf("big%d_%d" % (k, q)) for q in range(4)] for k in range(8)]
                bigall = [bb for row in bigb for bb in row]
                with contextlib.ExitStack() as sp_:
                    qg = [T(nc, sp_, "qg%d" % c, [128, S], BF16) for c in range(4)]
                    kz = [T(nc, sp_, "kz%d" % i, [128, S], BF16) for i in range(2)]
                    VgA = T(nc, sp_, "VgA", [128, NT, 2, 128], BF16)
                    cqn = T(nc, sp_, "cqn", [128, 2, S], BF16)
                    ckvn = T(nc, sp_, "ckvn", [128, 2, S], BF16)
                    kpe = T(nc, sp_, "kpe", [128, S], BF16)
                    cosM = T(nc, sp_, "cosM", [128, S], F32)
                    sinM = T(nc, sp_, "sinM", [128, S], F32)
                    w_uq = T(nc, sp_, "w_uq", [128, 2, 1024], BF16)
                    w_ukv = T(nc, sp_, "w_ukv", [128, 2, 1024], BF16)
                    with contextlib.ExitStack() as s0:
                        w_in = T(nc, s0, "w_in", [128, 8, 1312], BF16)
                        w_rot = T(nc, s0, "w_rot", [128, 8, 672], BF16)
                        xin = [T(nc, s0, "xin%d" % i, [128, D], F32) for i in range(2)]
                        cosG = T(nc, s0, "cosG", [128, S], F32)
                        sinG = T(nc, s0, "sinG", [128, S], F32)
                        sqb = [T(nc, s0, "sqb%d" % i, [128, 512], BF16) for i in range(4)]
                        rstd = [T(nc, s0, "rstd%d" % i, [128, 512], F32) for i in range(2)]
                        t1 = [T(nc, s0, "t1_%d" % i, [128, 512], F32) for i in range(2)]
                        t2 = [T(nc, s0, "t2_%d" % i, [128, 512], F32) for i in range(2)]
                        t3 = [T(nc, s0, "t3_%d" % i, [128, 512], F32) for i in range(2)]

                        w_in.pieces = [Buf("w_in_p%d" % i) for i in range(6)]
                        w_rot.pieces = [Buf("w_rot_p%d" % i) for i in range(6)]
                        w_in_v = w_in_d.rearrange("(k p) n -> p k n", p=128)
                        w_rot_v = w_rot_d.rearrange("(k p) n -> p k n", p=128)
                        for pc in range(6):
                            for (wt_, wv_, hi_, tag) in ((w_in, w_in_v, 1312, "wi"), (w_rot, w_rot_v, 672, "wr")):
                                c0_ = pc * 128
                                c1_ = c0_ + 128 if pc < 5 else hi_
                                sch.dma("pool", lambda q, wt_=wt_, wv_=wv_, c0_=c0_, c1_=c1_: q.dma_start(out=wt_.t[:, :, c0_:c1_], in_=wv_[:, :, c0_:c1_]),
                                        "%s%d" % (tag, pc), writes=[wt_.pieces[pc]])
                        cast_load(w_uq, w_uq.t[:], w_uq_d.rearrange("(k p) n -> p k n", p=128), "w2")
                        cast_load(w_ukv, w_ukv.t[:], w_ukv_d.rearrange("(k p) n -> p k n", p=128), "w3b")
                        for tt, ti in ((cosG, 0), (sinG, 1), (cosM, 2), (sinM, 3)):
                            sch.dma("sp", lambda q, tt=tt, ti=ti: q.dma_start(out=tt.t[:], in_=tabs_d[ti]),
                                    "tab%d" % ti, writes=[tt.b])
                        sch.op("pool", lambda g: g.memset(VgA.t[:], 1.0), writes=[VgA.b])
                        for _z in range(2):
                            sch.op("pool", lambda g, _z=_z: g.memset(kz[_z].t[:], 0.0), writes=[kz[_z].b])

                        def xload(i):
                            xi = xin[i % 2]
                            sch.dma("sp", lambda q, xi=xi, i=i: q.dma_start(out=xi.t[:], in_=x_d[b, i * 128:(i + 1) * 128, :]),
                                    "xin%d" % (i % 2), writes=[xi.b])
                            for hb in range(2):
                                bk = nbank()

                                def tr(pe, xi=xi, hb=hb, bk=bk):
                                    for k4 in range(4):
                                        ins = pe.transpose(out=ps[:, bk, k4 * 128:(k4 + 1) * 128],
                                                           in_=xi.t[:, (hb * 4 + k4) * 128:(hb * 4 + k4 + 1) * 128],
                                                           identity=identf.t[:])
                                    return ins
                                sch.op("pe", tr, reads=[xi.b, identf.b], writes=[PB[bk]])
                                dst = big.t[:, hb * 4:(hb + 1) * 4, i * 128:(i + 1) * 128]
                                src = ps[:, bk, :].rearrange("p (k c) -> p k c", k=4)
                                if hb == 0:
                                    sch.op("act", lambda a, dst=dst, src=src: a.copy(out=dst, in_=src),
                                           reads=[PB[bk]], accw=[bigb[k_][i // 4] for k_ in range(hb * 4, (hb + 1) * 4)])
                                else:
                                    sch.op("dve", lambda v, dst=dst, src=src: v.tensor_copy(out=dst, in_=src),
                                           reads=[PB[bk]], accw=[bigb[k_][i // 4] for k_ in range(hb * 4, (hb + 1) * 4)])

                        def mm_fm(bk, w_t, col0, M, nb, prow=0):
                            def f(pe):
                                for k in range(8):
                                    ins = pe.matmul(ps[prow:prow + M, bk, :], lhsT=w_t.t[:, k, col0:col0 + M],
                                                    rhs=big.t[:, k, nb * 512:(nb + 1) * 512],
                                                    start=(k == 0), stop=(k == 7))
                                return ins
                            sch.op("pe", f, reads=[w_t.pieces[min(col0 // 128, 5)]] + [bigb[k_][nb] for k_ in range(8)], writes=[PB[bk]])

                        it = 0
                        for c in range(5):
                            gi = 0 if c < 4 else 2
                            dst_t = qg[c] if c < 4 else None
                            for nb in range(4):
                                if c == 0:
                                    for i_ in range(nb * 4, nb * 4 + 4):
                                        xload(i_)
                                j = it % 2
                                it += 1
                                bA, bB, bC = nbank(), nbank(), nbank()
                                blk = slice(nb * 512, (nb + 1) * 512)
                                mm_fm(bA, w_in, c * 128, 128, nb)
                                mm_fm(bB, w_rot, c * 128, 128, nb)
                                sq = sqb[it % 4]
                                sch.op("act", lambda a, sq=sq, bA=bA: a.activation(out=sq.t[:], in_=ps[:, bA, :], func=AF.Square),
                                       reads=[PB[bA]], writes=[sq.b])
                                sch.op("pe", lambda pe, sq=sq, bC=bC: pe.matmul(ps[:, bC, :], lhsT=blockones, rhs=sq.t[:], start=True, stop=True),
                                       reads=[sq.b, cb.b], writes=[PB[bC]])
                                sch.op("act", lambda a, j=j, bC=bC: a.activation(out=rstd[j].t[:], in_=ps[:, bC, :], func=AF.Sqrt, bias=epsr.t[:, 0:1], scale=1.0),
                                       reads=[PB[bC], epsr.b], writes=[rstd[j].b])
                                sch.op("dve", lambda v, j=j: v.reciprocal(out=rstd[j].t[:], in_=rstd[j].t[:]),
                                       reads=[rstd[j].b], writes=[rstd[j].b])
                                sch.op("dve", lambda v, j=j, bA=bA, gi=gi, blk=blk: v.scalar_tensor_tensor(
                                    out=t1[j].t[:], in0=ps[:, bA, :], scalar=vecs.t[:, gi:gi + 1], in1=cosG.t[:, blk],
                                    op0=ALU.mult, op1=ALU.mult), reads=[PB[bA], vecs.b, cosG.b], writes=[t1[j].b])
                                sch.op("dve", lambda v, j=j, bB=bB, gi=gi, blk=blk: v.scalar_tensor_tensor(
                                    out=t2[j].t[:], in0=ps[:, bB, :], scalar=vecs.t[:, gi + 1:gi + 2], in1=sinG.t[:, blk],
                                    op0=ALU.mult, op1=ALU.mult), reads=[PB[bB], vecs.b, sinG.b], writes=[t2[j].b])
                                sch.op("pool", lambda g, j=j: g.tensor_tensor(out=t3[j].t[:], in0=t1[j].t[:], in1=t2[j].t[:], op=ALU.add),
                                       reads=[t1[j].b, t2[j].b], writes=[t3[j].b])
                                if dst_t is not None:
                                    sch.op("pool", lambda g, j=j, dst_t=dst_t, blk=blk: g.tensor_tensor(
                                        out=dst_t.t[:, blk], in0=t3[j].t[:], in1=rstd[j].t[:], op=ALU.mult),
                                        reads=[t3[j].b, rstd[j].b], accw=[dst_t.b])
                                else:
                                    for _z, rows in ((0, slice(0, 64)), (1, slice(64, 128))):
                                        sch.op("pool", lambda g, j=j, _z=_z, rows=rows, blk=blk: g.tensor_tensor(
                                            out=kz[_z].t[rows, blk], in0=t3[j].t[rows, :], in1=rstd[j].t[rows, :], op=ALU.mult),
                                            reads=[t3[j].b, rstd[j].b], accw=[kz[_z].b])

                        if b == 0 and stop == "A0b":
                            dump("qg0", qg[0])
                            stop_here("A0b")
                        for (dst_t, col0, gi) in ((cqn, 768, 4), (ckvn, 1024, 6)):
                            for nb in range(4):
                                j = it % 2
                                it += 1
                                blk = slice(nb * 512, (nb + 1) * 512)
                                bA0, bA1, bC = nbank(), nbank(), nbank()
                                mm_fm(bA0, w_in, col0, 128, nb)
                                mm_fm(bA1, w_in, col0 + 128, 128, nb)
                                s0_, s1_ = sqb[(2 * it) % 4], sqb[(2 * it + 1) % 4]
                                sch.op("act", lambda a, s0_=s0_, bA0=bA0: a.activation(out=s0_.t[:], in_=ps[:, bA0, :], func=AF.Square),
                                       reads=[PB[bA0]], writes=[s0_.b])
                                sch.op("act", lambda a, s1_=s1_, bA1=bA1: a.activation(out=s1_.t[:], in_=ps[:, bA1, :], func=AF.Square),
                                       reads=[PB[bA1]], writes=[s1_.b])

                                def msf(pe, s0_=s0_, s1_=s1_, bC=bC):
                                    pe.matmul(ps[:, bC, :], lhsT=ones256, rhs=s0_.t[:], start=True, stop=False)
                                    return pe.matmul(ps[:, bC, :], lhsT=ones256, rhs=s1_.t[:], start=False, stop=True)
                                sch.op("pe", msf, reads=[s0_.b, s1_.b, cb.b], writes=[PB[bC]])
                                sch.op("act", lambda a, j=j, bC=bC: a.activation(out=rstd[j].t[:], in_=ps[:, bC, :], func=AF.Sqrt, bias=epsr.t[:, 0:1], scale=1.0),
                                       reads=[PB[bC], epsr.b], writes=[rstd[j].b])
                                sch.op("dve", lambda v, j=j: v.reciprocal(out=rstd[j].t[:], in_=rstd[j].t[:]),
                                       reads=[rstd[j].b], writes=[rstd[j].b])
                                for kc, bA in ((0, bA0), (1, bA1)):
                                    sch.op("dve", lambda v, j=j, bA=bA, kc=kc, gi=gi, blk=blk, dst_t=dst_t: v.scalar_tensor_tensor(
                                        out=dst_t.t[:, kc, blk], in0=ps[:, bA, :], scalar=vecs.t[:, gi + kc:gi + kc + 1], in1=rstd[j].t[:],
                                        op0=ALU.mult, op1=ALU.mult), reads=[PB[bA], vecs.b, rstd[j].b], accw=[dst_t.b])

                        if b == 0 and stop == "A0c":
                            dump("cqn", cqn)
                            stop_here("A0c")
                        for nb in range(4):
                            j = it % 2
                            it += 1
                            blk = slice(nb * 512, (nb + 1) * 512)
                            bA, bB = nbank(), nbank()
                            mm_fm(bA, w_in, 1280, 32, nb, prow=64)
                            mm_fm(bB, w_rot, 640, 32, nb, prow=64)
                            sch.op("dve", lambda v, j=j, bA=bA, blk=blk: v.tensor_tensor(out=t1[j].t[64:96, :], in0=ps[64:96, bA, :], in1=cosM.t[64:96, blk], op=ALU.mult),
                                   reads=[PB[bA], cosM.b], writes=[t1[j].b])
                            sch.op("dve", lambda v, j=j, bB=bB, blk=blk: v.tensor_tensor(out=t2[j].t[64:96, :], in0=ps[64:96, bB, :], in1=sinM.t[64:96, blk], op=ALU.mult),
                                   reads=[PB[bB], sinM.b], writes=[t2[j].b])
                            sch.op("pool", lambda g, j=j, blk=blk: g.tensor_tensor(out=kpe.t[64:96, blk], in0=t1[j].t[64:96, :], in1=t2[j].t[64:96, :], op=ALU.add),
                                   reads=[t1[j].b, t2[j].b], accw=[kpe.b])

                        if b == 0 and stop == "A0d":
                            dump("kpe", kpe)
                            stop_here("A0d")
                        for i0 in range(0, NT, 4):
                            bk = nbank()

                            def vf(pe, i0=i0, bk=bk):
                                for jj in range(4):
                                    i = i0 + jj
                                    for k in range(8):
                                        ins = pe.matmul(ps[:, bk, jj * 128:(jj + 1) * 128], lhsT=big.t[:, k, i * 128:(i + 1) * 128],
                                                        rhs=w_in.t[:, k, 640:768], start=(k == 0), stop=(k == 7))
                                return ins
                            sch.op("pe", vf, reads=[w_in.pieces[5]] + [bigb[k_][i0 // 4] for k_ in range(8)], writes=[PB[bk]])
                            src = ps[:, bk, :].rearrange("p (i d) -> p i d", i=4)
                            sch.op("act", lambda a, i0=i0, src=src: a.copy(out=VgA.t[:, i0:i0 + 4, 0, 0:64], in_=src[:, :, 0:64]),
                                   reads=[PB[bk]], accw=[VgA.b])
                            sch.op("dve", lambda v, i0=i0, src=src: v.tensor_copy(out=VgA.t[:, i0:i0 + 4, 1, 64:128], in_=src[:, :, 64:128]),
                                   reads=[PB[bk]], accw=[VgA.b])
                        if b == 0 and stop == "A0":
                            for _c in range(4):
                                dump("qg%d" % _c, qg[_c])
                            dump("kg0", kz[0]); dump("kg1", kz[1]); dump("VgA", VgA); dump("cqn", cqn); dump("ckvn", ckvn); dump("kpe", kpe); dump("big", big, bigall)
                            stop_here("A0")
                        sch.flush()

                    w_out = T(nc, sp_, "w_out", [128, 8, D], BF16)
                    with contextlib.ExitStack() as s1:
                        mq = [T(nc, s1, "mq%d" % i, [128, S], BF16) for i in range(2)]
                        mk = [T(nc, s1, "mk%d" % i, [128, S], BF16) for i in range(2)]
                        mV = [T(nc, s1, "mV%d" % i, [128, NT, 128], BF16) for i in range(2)]
                        PT = [T(nc, s1, "PT%d" % i, [128, 1024], BF16) for i in range(3)]
                        acsb = [T(nc, s1, "acsb%d" % i, [128, 1024], F32) for i in range(2)]
                        dns = [T(nc, s1, "dns%d" % i, [128, 1024], F32) for i in range(2)]
                        scr = [T(nc, s1, "scr%d" % i, [128, 1024], F32) for i in range(2)]
                        nrm = [0]
                        m1 = [T(nc, s1, "m1_%d" % i, [128, 512], F32) for i in range(2)]
                        m2 = [T(nc, s1, "m2_%d" % i, [128, 512], F32) for i in range(2)]

                        cast_load(w_out, w_out.t[:], w_out_d.rearrange("(k p) n -> p k n", p=128), "w0")
                        for i in range(2):
                            sch.op("pool", lambda g, i=i: g.memset(mV[i].t[:], 1.0), writes=[mV[i].b])
                            sch.op("pool", lambda g, i=i: g.tensor_copy(out=mk[i].t[64:96, :], in_=kpe.t[64:96, :]),
                                   reads=[kpe.b], accw=[mk[i].b])

                        mctr = [0]
                        cur_step = [0]
                        deferred = {}

                        def defer(k, fn):
                            deferred.setdefault(cur_step[0] + k, []).append(fn)


                        def mla_prep_tasks(h):
                            bf = h % 2
                            tasks = []

                            def qk_task(nb):
                                blk = slice(nb * 512, (nb + 1) * 512)
                                j = mctr[0] % 2
                                mctr[0] += 1
                                bA, bB = 6, 7

                                def qf(pe):
                                    for kc in range(2):
                                        pe.matmul(ps[:, bA, :], lhsT=w_uq.t[:, kc, h * 128:(h + 1) * 128], rhs=cqn.t[:, kc, blk],
                                                  start=(kc == 0), stop=(kc == 1))
                                    for kc in range(2):
                                        ins = pe.matmul(ps[:, bB, :], lhsT=w_ukv.t[:, kc, h * 128:(h + 1) * 128], rhs=ckvn.t[:, kc, blk],
                                                        start=(kc == 0), stop=(kc == 1))
                                    return ins
                                sch.op("pe", qf, reads=[w_uq.b, w_ukv.b, cqn.b, ckvn.b], writes=[PB[bA], PB[bB]])

                                def evac():
                                    sch.op("dve", lambda v: v.tensor_copy(out=mq[bf].t[0:64, blk], in_=ps[0:64, bA, :]),
                                           reads=[PB[bA]], accw=[mq[bf].b])
                                    sch.op("dve", lambda v: v.tensor_tensor(out=m1[j].t[64:96, :], in0=ps[64:96, bA, :], in1=cosM.t[64:96, blk], op=ALU.mult),
                                           reads=[PB[bA], cosM.b], writes=[m1[j].b])
                                    sch.op("dve", lambda v: v.tensor_tensor(out=m2[j].t[96:128, :], in0=ps[96:128, bA, :], in1=sinM.t[96:128, blk], op=ALU.mult),
                                           reads=[PB[bA], sinM.b], writes=[m2[j].b])
                                    sch.op("pool", lambda g: g.tensor_copy(out=m2[j].t[64:96, :], in_=m2[j].t[96:128, :]),
                                           reads=[m2[j].b], accw=[m2[j].b])
                                    sch.op("pool", lambda g: g.tensor_tensor(out=mq[bf].t[64:96, blk], in0=m1[j].t[64:96, :], in1=m2[j].t[64:96, :], op=ALU.add),
                                           reads=[m1[j].b, m2[j].b], accw=[mq[bf].b])
                                    sch.op("dve", lambda v: v.tensor_copy(out=mk[bf].t[0:64, blk], in_=ps[0:64, bB, :]),
                                           reads=[PB[bB]], accw=[mk[bf].b])
                                defer(1, evac)

                            def v_task():
                                b0 = 6

                                def vf(pe):
                                    pv = ps2(b0)
                                    for i in range(NT):
                                        for kc in range(2):
                                            ins = pe.matmul(pv[:, i * 64:(i + 1) * 64], lhsT=ckvn.t[:, kc, i * 128:(i + 1) * 128],
                                                            rhs=w_ukv.t[:, kc, h * 128 + 64:h * 128 + 128], start=(kc == 0), stop=(kc == 1))
                                    return ins
                                sch.op("pe", vf, reads=[w_ukv.b, ckvn.b], writes=[PB[b0], PB[b0 + 1]])
                                off = 0 if h % 2 == 0 else 64
                                defer(1, lambda: sch.op("dve", lambda v: v.tensor_copy(out=mV[bf].t[:, :, off:off + 64],
                                                                                      in_=ps2(b0).rearrange("p (i d) -> p i d", d=64)),
                                                        reads=[PB[b0], PB[b0 + 1]], accw=[mV[bf].b]))
                            for nb in range(4):
                                tasks.append(lambda nb=nb: qk_task(nb))
                            tasks.append(v_task)
                            return tasks

                        heads = []
                        for jc in range(4):
                            heads.append(dict(q=qg[jc].t[:, :], k=kz[0].t[:, :], qb=qg[jc].b, kb=kz[0].b,
                                              va=(lambda i: VgA.t[:, i, 0, :]), vb=VgA.b, chunk=jc, odd=False, scale=64 ** -0.5, mla=None))
                            heads.append(dict(q=qg[jc].t[:, :], k=kz[1].t[:, :], qb=qg[jc].b, kb=kz[1].b,
                                              va=(lambda i: VgA.t[:, i, 1, :]), vb=VgA.b, chunk=jc, odd=True, scale=64 ** -0.5, mla=None))
                        for h in range(8):
                            bf = h % 2
                            heads.append(dict(q=mq[bf].t[0:96, :], k=mk[bf].t[0:96, :], qb=mq[bf].b, kb=mk[bf].b,
                                              va=(lambda i, bf=bf: mV[bf].t[:, i, :]), vb=mV[bf].b, chunk=4 + h // 2, odd=(h % 2 == 1),
                                              scale=96 ** -0.5, mla=h))
                        steps = []
                        for hi, hd in enumerate(heads):
                            for half in range(2):
                                for i in range(NT):
                                    steps.append((hi, hd, half, i))

                        sctr = [0]
                        accs = {}

                        def emit_qk(st):
                            hi, hd, half, i = st
                            if half == 0 and i == 0 and hd["mla"] is not None and hd["mla"] == 0:
                                pass
                            sb = 0 if (sctr[0] % 2 == 0) else 2
                            pt = PT[sctr[0] % 3]
                            sctr[0] += 1

                            def f(pe, hd=hd, half=half, i=i, sb=sb):
                                for c in range(2):
                                    ins = pe.matmul(ps[:, sb + c, :], lhsT=hd["k"][:, i * 128:(i + 1) * 128],
                                                    rhs=hd["q"][:, half * 1024 + c * 512: half * 1024 + (c + 1) * 512],
                                                    start=True, stop=True)
                                return ins
                            sch.op("pe", f, reads=[hd["qb"], hd["kb"]], writes=[PB[sb], PB[sb + 1]])
                            sch.op("act", lambda a, sb=sb, pt=pt, hd=hd: a.activation(out=pt.t[:], in_=ps2(sb), func=AF.Exp, scale=float(hd["scale"])),
                                   reads=[PB[sb], PB[sb + 1]], writes=[pt.b])
                            return pt

                        def emit_pv(st, pt):
                            hi, hd, half, i = st
                            key = (hi, half)
                            if key not in accs:
                                if hd["mla"] is None and hi < 7:
                                    accs[key] = 4 if (len(accs) % 2 == 0) else 6
                                else:
                                    accs[key] = 4
                            ab = accs[key]

                            def f(pe, hd=hd, i=i, ab=ab, pt=pt):
                                for c in range(2):
                                    ins = pe.matmul(ps[:, ab + c, :], lhsT=hd["va"](i), rhs=pt.t[:, c * 512:(c + 1) * 512],
                                                    start=(i == 0), stop=(i == NT - 1))
                                return ins
                            sch.op("pe", f, reads=[hd["vb"], pt.b], writes=[PB[ab], PB[ab + 1]] if i == 0 else (), accw=() if i == 0 else [PB[ab], PB[ab + 1]])
                            if i == NT - 1:
                                num = slice(64, 128) if hd["odd"] else slice(0, 64)
                                den = slice(0, 64) if hd["odd"] else slice(64, 128)
                                nrm[0] += 1
                                ac = acsb[nrm[0] % 2]
                                dn = dns[nrm[0] % 2]
                                sc_ = scr[nrm[0] % 2]
                                acc2 = ps2(ab)
                                sch.op("dve", lambda v, ac=ac, acc2=acc2: v.tensor_copy(out=ac.t[:], in_=acc2),
                                       reads=[PB[ab], PB[ab + 1]], writes=[ac.b])
                                sch.op("pool", lambda g, ac=ac, dn=dn, num=num, den=den: g.tensor_copy(out=dn.t[num, 0:512], in_=ac.t[den, 512:1024]),
                                       reads=[ac.b], accw=[dn.b])
                                sch.op("pool", lambda g, ac=ac, dn=dn, den=den: g.tensor_copy(out=dn.t[den, 0:512], in_=ac.t[den, 0:512]),
                                       reads=[ac.b], accw=[dn.b])
                                ch = hd["chunk"]
                                c0 = half * 1024

                                def nrm_a(dn=dn, sc_=sc_, num=num, den=den):
                                    sch.op("dve", lambda v: v.reciprocal(out=sc_.t[:, 0:512], in_=dn.t[:, 0:512]),
                                           reads=[dn.b], writes=[sc_.b])
                                    sch.op("pool", lambda g: g.tensor_copy(out=sc_.t[num, 512:1024], in_=sc_.t[den, 0:512]),
                                           reads=[sc_.b], accw=[sc_.b])

                                def nrm_b(ac=ac, sc_=sc_, num=num, ch=ch, c0=c0):
                                    sch.op("dve", lambda v: v.tensor_tensor(
                                        out=big.t[num, ch, c0 + 512:c0 + 1024], in0=ac.t[num, 512:1024], in1=sc_.t[num, 0:512], op=ALU.mult),
                                        reads=[ac.b, sc_.b], accw=[bigb[ch][c0 // 512 + 1]])
                                    sch.op("dve", lambda v: v.tensor_tensor(
                                        out=big.t[num, ch, c0:c0 + 512], in0=ac.t[num, 0:512], in1=sc_.t[num, 512:1024], op=ALU.mult),
                                        reads=[ac.b, sc_.b], accw=[bigb[ch][c0 // 512]])
                                defer(3, nrm_a)
                                defer(8, nrm_b)

                        pvq = []
                        pending = []
                        for si, st in enumerate(steps):
                            hi, hd, half, i = st
                            cur_step[0] = si
                            for fn in deferred.pop(si, []):
                                fn()
                            if half == 0 and i == 0:
                                nxt = hi + 1
                                if nxt < len(heads) and heads[nxt]["mla"] is not None:
                                    pending = mla_prep_tasks(heads[nxt]["mla"])
                            if pending and (i % 4 == 2):
                                pending.pop(0)()
                            if PRECAST and half == 0 and i == 8 and hi % 2 == 0:
                                e_ = b * (E // SPC) + hi // 2
                                sch.dma("pool", lambda q, e_=e_: q.dma_start(out=wdb_d[e_], in_=wd_d[e_]), "precast%d" % (e_ % 2),
                                        writes=[wdb_bufs[e_]])
                            pt = emit_qk(st)
                            pvq.append((st, pt))
                            if len(pvq) > 2:
                                emit_pv(*pvq.pop(0))
                        cur_step[0] = len(steps)
                        while pvq:
                            emit_pv(*pvq.pop(0))
                        for k in sorted(deferred):
                            for fn in deferred[k]:
                                fn()
                        deferred.clear()
                        if b == 0 and stop == "A1":
                            dump("big", big, bigall); dump("mq1", mq[1]); dump("mk1", mk[1]); dump("mV1", mV[1])
                            stop_here("A1")
                        sch.flush()

                    with contextlib.ExitStack() as s2:
                        def rt2(name, shape, dtype, depth):
                            return [T(nc, s2, "%s%d" % (name, i), shape, dtype) for i in range(depth)]

                        def R2(lst, n):
                            return lst[n % len(lst)]
                        w_r = T(nc, s2, "w_r", [128, 8, E], F32)
                        g1 = T(nc, s2, "g1", [128, D], F32)
                        b1 = T(nc, s2, "b1", [128, D], F32)
                        xin = rt2("xin2_", [128, D], F32, 3)
                        rr = rt2("rr", [128, D], F32, 4)
                        st_ = rt2("st", [128, 16], F32, 4)
                        xn = rt2("xn", [128, D], F32, 2)
                        x1 = rt2("x1_", [128, D], F32, 2)
                        accx = rt2("accx", [128, D], F32, 2)
                        x1b = rt2("x1b", [128, D], BF16, 2)
                        x1T = rt2("x1T", [128, 8, 128], F32, 2)
                        sm = rt2("sm", [128, 8], F32, 3)
                        ex = rt2("ex", [128, E], F32, 2)
                        aff = rt2("aff", [128, E], F32, 2)

                        sch.dma("sp", lambda q: q.dma_start(out=w_r.t[:], in_=w_r_d.rearrange("(k p) n -> p k n", p=128)), "w3", writes=[w_r.b])
                        sch.dma("sp", lambda q: q.dma_start(out=g1.t[:], in_=lnp_d[0]), "ln0", writes=[g1.b])
                        sch.dma("sp", lambda q: q.dma_start(out=b1.t[:], in_=lnp_d[1]), "ln1", writes=[b1.b])

                        def sa(src_t, stt):
                            def f(v):
                                v.bn_stats(out=stt.t[:, 0:6], in_=src_t.t[:, 0:512])
                                return v.bn_stats(out=stt.t[:, 6:12], in_=src_t.t[:, 512:1024])
                            sch.op("dve", f, reads=[src_t.b], writes=[stt.b])
                            sch.op("dve", lambda v: v.bn_aggr(out=stt.t[:, 12:14], in_=stt.t[:, 0:12]), reads=[stt.b], accw=[stt.b])

                        def a0(i):
                            tok = slice(i * 128, (i + 1) * 128)
                            xi = R2(xin, i)
                            sch.dma("sp", lambda q: q.dma_start(out=xi.t[:], in_=x_d[b, tok, :]), "xin%d" % (i % len(xin)), writes=[xi.b])
                            b0 = 0 if i % 2 == 0 else 2

                            def of(pe):
                                for hf in range(2):
                                    for k in range(8):
                                        ins = pe.matmul(ps[:, b0 + hf, :], lhsT=big.t[:, k, tok], rhs=w_out.t[:, k, hf * 512:(hf + 1) * 512],
                                                        start=(k == 0), stop=(k == 7))
                                return ins
                            sch.op("pe", of, reads=[w_out.b] + [bigb[k_][i // 4] for k_ in range(8)], writes=[PB[b0], PB[b0 + 1]])

                        def a1(i):
                            xi, r_, stt = R2(xin, i), R2(rr, i), R2(st_, i)
                            b0 = 0 if i % 2 == 0 else 2
                            sch.op("dve", lambda v: v.scalar_tensor_tensor(out=r_.t[:], in0=xi.t[:], scalar=ALPHA, in1=ps2(b0),
                                                                           op0=ALU.mult, op1=ALU.add),
                                   reads=[xi.b, PB[b0], PB[b0 + 1]], writes=[r_.b])
                            sa(r_, stt)

                        def a2_(i):
                            stt = R2(st_, i)
                            sch.op("act", lambda a: a.activation(out=stt.t[:, 14:15], in_=stt.t[:, 13:14], func=AF.Sqrt, bias=epsr.t[:, 1:2], scale=1.0),
                                   reads=[stt.b, epsr.b], accw=[stt.b])

                        def a3(i):
                            stt = R2(st_, i)
                            sch.op("dve", lambda v: v.reciprocal(out=stt.t[:, 14:15], in_=stt.t[:, 14:15]), reads=[stt.b], accw=[stt.b])
                            sch.op("dve", lambda v: v.scalar_tensor_tensor(out=stt.t[:, 15:16], in0=stt.t[:, 12:13], scalar=-1.0, in1=stt.t[:, 14:15],
                                                                           op0=ALU.mult, op1=ALU.mult), reads=[stt.b], accw=[stt.b])

                        def a4(i):
                            r_, stt, xn_ = R2(rr, i), R2(st_, i), R2(xn, i)
                            sch.op("act", lambda a: a.activation(out=xn_.t[:], in_=r_.t[:], func=AF.Identity, bias=stt.t[:, 15:16], scale=stt.t[:, 14:15]),
                                   reads=[r_.b, stt.b], writes=[xn_.b])

                        def a5(i):
                            xn_, x1_ = R2(xn, i), R2(x1, i)
                            sch.op("pool", lambda g: g.tensor_tensor(out=xn_.t[:], in0=xn_.t[:], in1=g1.t[:], op=ALU.mult), reads=[xn_.b, g1.b], writes=[xn_.b])
                            sch.op("pool", lambda g: g.tensor_tensor(out=x1_.t[:], in0=xn_.t[:], in1=b1.t[:], op=ALU.add), reads=[xn_.b, b1.b], writes=[x1_.b])

                        def a6(i):
                            x1_, ax, xb = R2(x1, i), R2(accx, i), R2(x1b, i)
                            row0 = b * S + i * 128
                            sch.op("act", lambda a: a.activation(out=ax.t[:], in_=x1_.t[:], func=AF.Copy, scale=ALPHA), reads=[x1_.b], writes=[ax.b])
                            sch.dma("sp", lambda q: q.dma_start(out=acc_d[row0:row0 + 128, :], in_=ax.t[:]), "accst%d" % (i % 2),
                                    reads=[ax.b], accw=[acc_bufs[b]])
                            sch.op("act", lambda a: a.copy(out=xb.t[:], in_=x1_.t[:]), reads=[x1_.b], writes=[xb.b])
                            sch.dma("sp", lambda q: q.dma_start(out=x1b_d[row0:row0 + 128, :], in_=xb.t[:]), "x1bst%d" % (i % 2),
                                    reads=[xb.b], accw=[x1b_buf])

                            def trf(pe):
                                pv = ps2(4)
                                for k in range(8):
                                    ins = pe.transpose(out=pv[:, k * 128:(k + 1) * 128], in_=x1_.t[:, k * 128:(k + 1) * 128], identity=identf.t[:])
                                return ins
                            sch.op("pe", trf, reads=[x1_.b, identf.b], writes=[PB[4], PB[5]])

                        def a7(i):
                            xt_ = R2(x1T, i)
                            sch.op("dve", lambda v: v.tensor_copy(out=xt_.t[:].rearrange("p k c -> p (k c)"), in_=ps2(4)),
                                   reads=[PB[4], PB[5]], writes=[xt_.b])

                        def a8(i):
                            xt_ = R2(x1T, i)
                            bl = 6 + i % 2

                            def lf(pe):
                                for k in range(8):
                                    ins = pe.matmul(ps[:, bl, 0:E], lhsT=xt_.t[:, k, :], rhs=w_r.t[:, k, :], start=(k == 0), stop=(k == 7))
                                return ins
                            sch.op("pe", lf, reads=[xt_.b, w_r.b], writes=[PB[bl]])

                        def a9(i):
                            s_ = R2(sm, i)
                            bl = 6 + i % 2
                            sch.op("dve", lambda v: v.reduce_max(out=s_.t[:, 0:1], in_=ps[:, bl, 0:E], axis=AX.X), reads=[PB[bl]], writes=[s_.b])
                            sch.op("dve", lambda v: v.tensor_scalar_mul(out=s_.t[:, 1:2], in0=s_.t[:, 0:1], scalar1=-1.0), reads=[s_.b], accw=[s_.b])

                        def a10(i):
                            s_, e_ = R2(sm, i), R2(ex, i)
                            bl = 6 + i % 2
                            sch.op("act", lambda a: a.activation(out=e_.t[:], in_=ps[:, bl, 0:E], func=AF.Exp, bias=s_.t[:, 1:2], scale=1.0,
                                                                 accum_out=s_.t[:, 2:3]),
                                   reads=[PB[bl], s_.b], writes=[e_.b], accw=[s_.b])

                        def a11(i):
                            s_, e_, af = R2(sm, i), R2(ex, i), R2(aff, i)
                            sch.op("dve", lambda v: v.reciprocal(out=s_.t[:, 3:4], in_=s_.t[:, 2:3]), reads=[s_.b, e_.b], accw=[s_.b])
                            sch.op("dve", lambda v: v.tensor_scalar_mul(out=af.t[:], in0=e_.t[:], scalar1=s_.t[:, 3:4]), reads=[e_.b, s_.b], writes=[af.b])

                        def a12(i):
                            af = R2(aff, i)
                            bl = 6 + i % 2
                            sch.op("pe", lambda pe: pe.transpose(out=ps[0:E, bl, 128:256], in_=af.t[:], identity=identf.t[:]),
                                   reads=[af.b, identf.b], writes=[PB[bl]])

                        def a13(i):
                            tok = slice(i * 128, (i + 1) * 128)
                            bl = 6 + i % 2
                            sch.op("dve", lambda v: v.tensor_copy(out=affT.t[32 * b:32 * b + E, tok], in_=ps[0:E, bl, 128:256]),
                                   reads=[PB[bl]], accw=[affT.b])

                        pipeline(NT, [a0, a1, a2_, a3, a4, a5, a6, a7, a8, a9, a10, a11, a12, a13])
                        if b == 0 and stop == "A2":
                            dump("affT", affT)
                            stop_here("A2")
                        sch.flush()

        if True:
            st = top
            work = T(nc, st, "tk_work", [48, S], F32)
            vals = T(nc, st, "tk_vals", [48, CAP], F32)
            idxu = T(nc, st, "tk_idxu", [48, CAP], U32)
            idxf = T(nc, st, "tk_idxf", [48, CAP], F32)
            for r in range(CAP // 8):
                sl = slice(r * 8, (r + 1) * 8)
                src = affT if r == 0 else work
                sch.op("dve", lambda v, sl=sl, src=src: v.max(out=vals.t[:, sl], in_=src.t[:]), reads=[src.b], accw=[vals.b])
                sch.op("dve", lambda v, sl=sl, src=src: v.max_index(out=idxu.t[:, sl], in_max=vals.t[:, sl], in_values=src.t[:]),
                       reads=[src.b, vals.b], accw=[idxu.b])
                if r < CAP // 8 - 1:
                    sch.op("dve", lambda v, sl=sl, src=src: v.match_replace(out=work.t[:], in_to_replace=vals.t[:, sl], in_values=src.t[:], imm_value=-1.0),
                           reads=[src.b, vals.b], writes=[work.b])
            sch.op("dve", lambda v: v.tensor_copy(out=idxf.t[:], in_=idxu.t[:]), reads=[idxu.b], writes=[idxf.b])
            sch.op("dve", lambda v: v.tensor_scalar_add(out=idxf.t[32:48, :], in0=idxf.t[32:48, :], scalar1=float(S)), reads=[idxf.b], accw=[idxf.b])
            for ct in range(2):
                bk = nbank()
                sch.op("pe", lambda pe, ct=ct, bk=bk: pe.transpose(out=ps[:, bk, 0:48], in_=vals.t[:, ct * 128:(ct + 1) * 128], identity=identf.t[0:48, 0:48]),
                       reads=[vals.b, identf.b], writes=[PB[bk]])
                sch.op("dve", lambda v, ct=ct, bk=bk: v.tensor_copy(out=gT.t[:, ct, :], in_=ps[:, bk, 0:48]), reads=[PB[bk]], accw=[gT.b])
                bk2 = nbank()
                sch.op("pe", lambda pe, ct=ct, bk2=bk2: pe.transpose(out=ps[:, bk2, 0:48], in_=idxf.t[:, ct * 128:(ct + 1) * 128], identity=identf.t[0:48, 0:48]),
                       reads=[idxf.b, identf.b], writes=[PB[bk2]])
                sch.op("dve", lambda v, ct=ct, bk2=bk2: v.tensor_copy(out=idxT.t[:, ct, :], in_=ps[:, bk2, 0:48]), reads=[PB[bk2]], accw=[idxT.b])
            if stop == "T":
                dump("affT", affT); dump("gT", gT); dump("idxT", idxT); dump("vals", vals); dump("idxu", idxu)
                stop_here("T")

        with contextlib.ExitStack() as sb_:
            ring = [T(nc, sb_, "ring%d" % i, [128, 4096], BF16) for i in range(NSLOT)]
            wdt = [[T(nc, sb_, "wd%d_%d" % (hf, i), [128, 4, 512], BF16) for i in range(6)] for hf in range(2)]
            xg = [T(nc, sb_, "xg%d" % i, [128, 4, D], BF16) for i in range(2)]
            xgT = [T(nc, sb_, "xgT%d" % i, [128, 8, 512], BF16) for i in range(2)]
            hT = T(nc, sb_, "hT", [128, NFC, 512], BF16)
            sg = [T(nc, sb_, "sg%d" % i, [128, 512], F32) for i in range(2)]
            gy = [T(nc, sb_, "gy%d" % i, [128, D], F32) for i in range(4)]

            loads = []
            for e in range(E):
                for kind, fb in ([(k, f) for f in range(5) for k in ("g", "u")] + [("d0", f) for f in range(6)] + [("g", 5), ("u", 5)]
                                 + [("d1", f) for f in range(6)]):
                    loads.append((kind, e, fb))
            ring_use = [0]
            ring_done = [0]
            p2_done = [-1]
            p2a_done = [-1]
            ring_of = {}

            def issue_load(ld):
                kind, e, fb = ld
                nf = 512 if fb < 5 else 256
                if kind in ("d0", "d1"):
                    nch = nf // 128
                    hf = int(kind[1])
                    dst = wdt[hf][fb]
                    sch.dma("sp", lambda q, dst=dst, e=e, fb=fb, nch=nch, hf=hf: q.dma_start(
                        out=dst.t[:, 0:nch, :],
                        in_=wdb_d[e, fb * 512:fb * 512 + nch * 128, hf * 512:(hf + 1) * 512].rearrange("(c p) n -> p c n", p=128)),
                        "wd%d_%d" % (hf, fb), reads=[wdb_bufs[e]], writes=[dst.b])
                else:
                    si = ring_use[0] % NSLOT
                    slot = ring[si]
                    ring_of[(kind, e, fb)] = slot
                    ring_use[0] += 1
                    srcw = wg_d if kind == "g" else wu_d
                    sch.dma("pool", lambda q, slot=slot, e=e, fb=fb, nf=nf, srcw=srcw: q.dma_start(
                        out=slot.t[:, 0:8 * nf].rearrange("p (k f) -> p k f", k=8),
                        in_=srcw[e, :, fb * 512:fb * 512 + nf].rearrange("(k p) f -> p k f", p=128)),
                        "ring%d" % si, writes=[slot.b])

            lptr = [0]

            def pump():
                while lptr[0] < len(loads):
                    kind, e, fb = loads[lptr[0]]
                    if kind == "d0":
                        if e - 1 > p2a_done[0]:
                            break
                    elif kind == "d1":
                        if e - 1 > p2_done[0]:
                            break
                    else:
                        if ring_use[0] - ring_done[0] >= NSLOT:
                            break
                    issue_load(loads[lptr[0]])
                    lptr[0] += 1

            def gather(e):
                xb = xg[e % 2]
                for b in range(SPC):
                    for ct in range(2):
                        col = 32 * b + e
                        sch.dma("pool", lambda q, xb=xb, b=b, ct=ct, col=col: q.indirect_dma_start(
                            out=xb.t[:, b * 2 + ct, :], out_offset=None, in_=x1b_d[:, :],
                            in_offset=bass.IndirectOffsetOnAxis(ap=idxT.t[:, ct, col:col + 1], axis=0)),
                            "xg%d_%d" % (e % 2, b * 2 + ct), reads=[x1b_buf, idxT.b], accw=[xb.b])

            def transpose_xg(e):
                xb = xg[e % 2]
                xt = xgT[e % 2]
                for cc in range(4):
                    bk = nbank()

                    def f(pe, xb=xb, cc=cc, bk=bk):
                        pv = ps[:, bk, :].bitcast(BF16)
                        for k in range(8):
                            ins = pe.transpose(out=pv[:, k * 128:(k + 1) * 128], in_=xb.t[:, cc, k * 128:(k + 1) * 128], identity=identb)
                        return ins
                    sch.op("pe", f, reads=[xb.b, cb.b], writes=[PB[bk]])
                    eng = "act" if cc % 2 == 0 else "dve"
                    src = ps[:, bk, :].bitcast(BF16).rearrange("p (k c) -> p k c", k=8)
                    dst = xt.t[:, :, cc * 128:(cc + 1) * 128]
                    if eng == "act":
                        sch.op("act", lambda a, dst=dst, src=src: a.copy(out=dst, in_=src), reads=[PB[bk]], accw=[xt.b])
                    else:
                        sch.op("dve", lambda v, dst=dst, src=src: v.tensor_copy(out=dst, in_=src), reads=[PB[bk]], accw=[xt.b])

            pump()
            gather(0)
            transpose_xg(0)
            fctr = 0
            for e in range(E):
                xt = xgT[e % 2]
                if e + 1 < E:
                    gather(e + 1)
                for fc in range(NFC):
                    fb, jf = fc // 4, fc % 4
                    nf = 512 if fb < 5 else 256
                    pump()
                    assert ("u", e, fb) in ring_of, (e, fb, lptr[0])
                    sg_slot = ring_of[("g", e, fb)]
                    su_slot = ring_of[("u", e, fb)]
                    bG, bU = nbank(), nbank()

                    def gf(pe, slot=sg_slot, bk=bG, jf=jf, nf=nf, xt=xt):
                        wv = slot.t[:, 0:8 * nf].rearrange("p (k f) -> p k f", k=8)
                        for k in range(8):
                            ins = pe.matmul(ps[:, bk, :], lhsT=wv[:, k, jf * 128:(jf + 1) * 128], rhs=xt.t[:, k, :], start=(k == 0), stop=(k == 7))
                        return ins
                    sch.op("pe", gf, reads=[sg_slot.b, xt.b], writes=[PB[bG]])
                    sch.op("pe", lambda pe, slot=su_slot, bk=bU, jf=jf, nf=nf, xt=xt: gf(pe, slot, bk, jf, nf, xt), reads=[su_slot.b, xt.b], writes=[PB[bU]])
                    j = fctr % 2
                    fctr += 1
                    sch.op("act", lambda a, j=j, bG=bG: a.activation(out=sg[j].t[:], in_=ps[:, bG, :], func=AF.Silu), reads=[PB[bG]], writes=[sg[j].b])
                    sch.op("dve", lambda v, j=j, bU=bU, fc=fc: v.tensor_tensor(out=hT.t[:, fc, :], in0=ps[:, bU, :], in1=sg[j].t[:], op=ALU.mult),
                           reads=[PB[bU], sg[j].b], accw=[hT.b])
                    if jf == 3 or fc == NFC - 1:
                        ring_done[0] += 2
                        pump()
                if e + 1 < E:
                    transpose_xg(e + 1)
                for hf in range(2):
                    for b in range(SPC):
                        for ct in range(2):
                            cc = b * 2 + ct
                            col = 32 * b + e
                            gyt = gy[cc]
                            bk = nbank()

                            def df(pe, cc=cc, hf=hf, bk=bk):
                                for fc in range(NFC):
                                    ins = pe.matmul(ps[:, bk, :], lhsT=hT.t[:, fc, cc * 128:(cc + 1) * 128],
                                                    rhs=wdt[hf][fc // 4].t[:, fc % 4, :], start=(fc == 0), stop=(fc == NFC - 1))
                                return ins
                            sch.op("pe", df, reads=[hT.b] + [w.b for w in wdt[hf]], writes=[PB[bk]])
                            if cc % 2 == 0:
                                sch.op("act", lambda a, gyt=gyt, bk=bk, ct=ct, col=col, hf=hf: a.activation(
                                    out=gyt.t[:, hf * 512:(hf + 1) * 512], in_=ps[:, bk, :], func=AF.Copy, scale=gT.t[:, ct, col:col + 1]),
                                    reads=[PB[bk], gT.b], writes=[gyt.b] if hf == 0 else (), accw=() if hf == 0 else [gyt.b])
                            else:
                                sch.op("dve", lambda v, gyt=gyt, bk=bk, ct=ct, col=col, hf=hf: v.tensor_scalar_mul(
                                    out=gyt.t[:, hf * 512:(hf + 1) * 512], in0=ps[:, bk, :], scalar1=gT.t[:, ct, col:col + 1]),
                                    reads=[PB[bk], gT.b], writes=[gyt.b] if hf == 0 else (), accw=() if hf == 0 else [gyt.b])
                            if hf == 1:
                                sch.dma("pool", lambda q, gyt=gyt, ct=ct, col=col: q.indirect_dma_start(
                                    out=acc_d[:, :], out_offset=bass.IndirectOffsetOnAxis(ap=idxT.t[:, ct, col:col + 1], axis=0),
                                    in_=gyt.t[:, :], in_offset=None, compute_op=ALU.add),
                                    "scat%d" % cc, reads=[gyt.b, idxT.b], writes=[acc_bufs[b]])
                    if hf == 0:
                        p2a_done[0] = e
                        pump()
                p2_done[0] = e
                pump()
            if stop == "B":
                stop_here("B")
            sch.flush()

        with contextlib.ExitStack() as sc:
            def rt(name, shape, dtype, depth):
                return [T(nc, sc, "%s%d" % (name, i), shape, dtype) for i in range(depth)]
            wpg = T(nc, sc, "wpg", [128, 8, D], BF16)
            wpp = T(nc, sc, "wpp", [128, 2, D], BF16)
            lnt = [T(nc, sc, "lnt%d" % i, [128, D], F32) for i in range(4)]
            acc_t = rt("acc_t", [128, D], F32, 5)
            pin = rt("pin", [128, PLE], F32, 2)
            stc = rt("stc", [128, 16], F32, 4)
            pT = rt("pT", [128, 2, 128], BF16, 8)
            tmp = rt("tmpc", [128, D], F32, 2)
            x2 = rt("x2_", [128, D], F32, 7)
            x2b = rt("x2b", [128, D], BF16, 2)
            x2T = rt("x2T", [128, 8, 128], BF16, 2)
            sgm = rt("sgm", [128, D], F32, 2)
            r3 = rt("r3_", [128, D], F32, 4)
            std = rt("std", [128, 16], F32, 4)
            tmp3 = rt("tmp3c", [128, D], F32, 3)
            yo = rt("yo", [128, D], F32, 2)

            def R(lst, n):
                return lst[n % len(lst)]

            cast_load(wpg, wpg.t[:], wpg_d.rearrange("(k p) n -> p k n", p=128), "w0")
            cast_load(wpp, wpp.t[:], wpp_d.rearrange("(k p) n -> p k n", p=128), "w1")
            for i in range(4):
                sch.dma("sp", lambda q, i=i: q.dma_start(out=lnt[i].t[:], in_=lnp_d[2 + i]), "ln%d" % i, writes=[lnt[i].b])

            def st_a(src_t, stt):
                def f(v):
                    v.bn_stats(out=stt.t[:, 0:6], in_=src_t.t[:, 0:512])
                    return v.bn_stats(out=stt.t[:, 6:12], in_=src_t.t[:, 512:1024])
                sch.op("dve", f, reads=[src_t.b], writes=[stt.b])
                sch.op("dve", lambda v: v.bn_aggr(out=stt.t[:, 12:14], in_=stt.t[:, 0:12]), reads=[stt.b], accw=[stt.b])

            def st_b(stt, ec=1):
                sch.op("act", lambda a: a.activation(out=stt.t[:, 14:15], in_=stt.t[:, 13:14], func=AF.Sqrt, bias=epsr.t[:, ec:ec + 1], scale=1.0),
                       reads=[stt.b, epsr.b], accw=[stt.b])

            def st_c(stt):
                sch.op("dve", lambda v: v.reciprocal(out=stt.t[:, 14:15], in_=stt.t[:, 14:15]), reads=[stt.b], accw=[stt.b])
                sch.op("dve", lambda v: v.scalar_tensor_tensor(out=stt.t[:, 15:16], in0=stt.t[:, 12:13], scalar=-1.0, in1=stt.t[:, 14:15],
                                                               op0=ALU.mult, op1=ALU.mult), reads=[stt.b], accw=[stt.b])

            def c0(n):
                b, i = divmod(n, NT)
                tok = slice(i * 128, (i + 1) * 128)
                row0 = b * S + i * 128
                at, pi = R(acc_t, n), R(pin, n)
                sch.dma("sp", lambda q: q.dma_start(out=at.t[:], in_=acc_d[row0:row0 + 128, :]), "acct%d" % (n % len(acc_t)),
                        reads=[acc_bufs[b]], writes=[at.b])
                sch.dma("sp", lambda q: q.dma_start(out=pi.t[:], in_=p_d[b, tok, :]), "pin%d" % (n % len(pin)), writes=[pi.b])

            def c1(n):
                st_a(R(acc_t, n), R(stc, n))
                bp = n % 2
                pi = R(pin, n)

                def ptf(pe):
                    for k in range(2):
                        ins = pe.transpose(out=ps[:, bp, k * 128:(k + 1) * 128], in_=pi.t[:, k * 128:(k + 1) * 128], identity=identf.t[:])
                    return ins
                sch.op("pe", ptf, reads=[pi.b, identf.b], writes=[PB[bp]])

            def c2(n):
                st_b(R(stc, n))
                bp = n % 2
                pt_ = R(pT, n)
                sch.op("act", lambda a: a.copy(out=pt_.t[:].rearrange("p k c -> p (k c)"), in_=ps[:, bp, 0:256]),
                       reads=[PB[bp]], writes=[pt_.b])

            def c3(n):
                st_c(R(stc, n))

            def c4(n):
                at, stt, tm = R(acc_t, n), R(stc, n), R(tmp, n)
                sch.op("act", lambda a: a.activation(out=tm.t[:], in_=at.t[:], func=AF.Identity, bias=stt.t[:, 15:16], scale=stt.t[:, 14:15]),
                       reads=[at.b, stt.b], writes=[tm.b])

            def c5(n):
                tm, xx = R(tmp, n), R(x2, n)
                sch.op("pool", lambda g: g.tensor_tensor(out=tm.t[:], in0=tm.t[:], in1=lnt[0].t[:], op=ALU.mult), reads=[tm.b, lnt[0].b], writes=[tm.b])
                sch.op("pool", lambda g: g.tensor_tensor(out=xx.t[:], in0=tm.t[:], in1=lnt[1].t[:], op=ALU.add), reads=[tm.b, lnt[1].b], writes=[xx.b])

            def c6(n):
                xx, xb = R(x2, n), R(x2b, n)
                sch.op("act", lambda a: a.copy(out=xb.t[:], in_=xx.t[:]), reads=[xx.b], writes=[xb.b])

            def c7(n):
                xb = R(x2b, n)
                bk = 2 + n % 2

                def trf(pe):
                    pv = ps[:, bk, :].bitcast(BF16)
                    for k in range(8):
                        ins = pe.transpose(out=pv[:, k * 128:(k + 1) * 128], in_=xb.t[:, k * 128:(k + 1) * 128], identity=identb)
                    return ins
                sch.op("pe", trf, reads=[xb.b, cb.b], writes=[PB[bk]])

            def c8(n):
                bk = 2 + n % 2
                xt_ = R(x2T, n)
                sch.op("act", lambda a: a.copy(out=xt_.t[:].rearrange("p k c -> p (k c)"), in_=ps[:, bk, :].bitcast(BF16)),
                       reads=[PB[bk]], writes=[xt_.b])

            def c9(n):
                xt_ = R(x2T, n)

                def gf(pe):
                    for hf in range(2):
                        for k in range(8):
                            ins = pe.matmul(ps[:, 4 + hf, :], lhsT=xt_.t[:, k, :], rhs=wpg.t[:, k, hf * 512:(hf + 1) * 512],
                                            start=(k == 0), stop=(k == 7))
                    return ins
                sch.op("pe", gf, reads=[xt_.b, wpg.b], writes=[PB[4], PB[5]])

            def c10(n):
                pt_, sg_ = R(pT, n), R(sgm, n)
                sch.op("act", lambda a: a.activation(out=sg_.t[:], in_=ps2(4), func=AF.Sigmoid), reads=[PB[4], PB[5]], writes=[sg_.b])

                def ef(pe):
                    for hf in range(2):
                        for k in range(2):
                            ins = pe.matmul(ps[:, 6 + hf, :], lhsT=pt_.t[:, k, :], rhs=wpp.t[:, k, hf * 512:(hf + 1) * 512],
                                            start=(k == 0), stop=(k == 1))
                    return ins
                sch.op("pe", ef, reads=[pt_.b, wpp.b], writes=[PB[6], PB[7]])

            def c11(n):
                sg_, xx, rr_, sd = R(sgm, n), R(x2, n), R(r3, n), R(std, n)
                sch.op("dve", lambda v: v.scalar_tensor_tensor(out=sg_.t[:], in0=ps2(6), scalar=1.0 / ALPHA, in1=sg_.t[:],
                                                               op0=ALU.mult, op1=ALU.mult),
                       reads=[PB[6], PB[7], sg_.b], writes=[sg_.b])
                sch.op("dve", lambda v: v.tensor_tensor(out=rr_.t[:], in0=xx.t[:], in1=sg_.t[:], op=ALU.add), reads=[xx.b, sg_.b], writes=[rr_.b])
                st_a(rr_, sd)

            def c12(n):
                st_b(R(std, n), ec=2)

            def c13(n):
                st_c(R(std, n))

            def c14(n):
                rr_, sd, t3_ = R(r3, n), R(std, n), R(tmp3, n)
                sch.op("act", lambda a: a.activation(out=t3_.t[:], in_=rr_.t[:], func=AF.Identity, bias=sd.t[:, 15:16], scale=sd.t[:, 14:15]),
                       reads=[rr_.b, sd.b], writes=[t3_.b])

            def c15(n):
                t3_ = R(tmp3, n)
                sch.op("dve", lambda v: v.tensor_tensor(out=t3_.t[:], in0=t3_.t[:], in1=lnt[2].t[:], op=ALU.mult), reads=[t3_.b, lnt[2].b], writes=[t3_.b])

            def c16(n):
                b, i = divmod(n, NT)
                tok = slice(i * 128, (i + 1) * 128)
                t3_, yy = R(tmp3, n), R(yo, n)
                sch.op("pool", lambda g: g.tensor_tensor(out=yy.t[:], in0=t3_.t[:], in1=lnt[3].t[:], op=ALU.add), reads=[t3_.b, lnt[3].b], writes=[yy.b])
                sch.dma("sp", lambda q: q.dma_start(out=out_d[b, tok, :], in_=yy.t[:]), "outst%d" % (n % len(yo)), reads=[yy.b])

            pipeline(SPC * NT, [c0, c1, c2, c3, c4, c5, c6, c7, c8, c9, c10, c11, c12, c13, c14, c15, c16])
            sch.flush()

    except StopBuild:
        pass
    nc._dumps = dumps
    return nc


def _rope_tables():
    t = np.arange(S)
    row = (t // 64).astype(np.float32)
    col = (t % 64).astype(np.float32)
    tabs = np.zeros((4, 128, S), np.float32)
    inv16 = (10000.0 ** (-np.arange(16, dtype=np.float32) * 2.0 / 32.0)).astype(np.float32)
    for p in range(128):
        d = p % 64
        blk, f = d // 16, d % 16
        pos = row if blk < 2 else col
        ang = (pos * inv16[f]).astype(np.float32)
        tabs[0, p] = np.cos(ang)
        tabs[1, p] = np.sin(ang) * (-1.0 if blk in (0, 2) else 1.0)
    tabs[2, :, :] = 1.0
    inv8 = (10000.0 ** (-np.arange(8, dtype=np.float32) * 2.0 / 16.0)).astype(np.float32)
    for p in range(64, 96):
        d = p - 64
        blk, f = d // 8, d % 8
        pos = row if blk < 2 else col
        ang = (pos * inv8[f]).astype(np.float32)
        tabs[2, p] = np.cos(ang)
        tabs[3, p] = np.sin(ang) * (-1.0 if blk in (0, 2) else 1.0)
    tabs[3, 96:128] = tabs[3, 64:96]
    return tabs


_PERM64 = np.concatenate([np.arange(16, 32), np.arange(0, 16), np.arange(48, 64), np.arange(32, 48)])
_PERM32 = np.concatenate([np.arange(8, 16), np.arange(0, 8), np.arange(24, 32), np.arange(16, 24)])


def _prep_shared(inp):
    f32 = np.float32
    w_in = np.asarray(inp["w_in"][0], f32)
    qcols = []
    for j in range(4):
        qcols += list(range(j * 64, (j + 1) * 64)) + list(range((j + 4) * 64, (j + 5) * 64))
    cols = np.array(qcols + list(range(512, 1312)))
    w_in_p = np.ascontiguousarray(w_in[:, cols])
    rot_cols = []
    for hblk in range(10):
        rot_cols += list(hblk * 64 + _PERM64)
    w_in_rot = np.ascontiguousarray(np.concatenate([w_in_p[:, np.array(rot_cols)], w_in[:, 1280 + _PERM32]], axis=1))
    w_uq = np.asarray(inp["w_uq"][0], f32)
    rc = []
    for h in range(8):
        rc += list(range(h * 96, (h + 1) * 96)) + list(h * 96 + 64 + _PERM32)
    w_uq_cat = np.ascontiguousarray(w_uq[:, np.array(rc)])
    w_out = np.asarray(inp["w_out"][0], f32)
    rows = []
    for j in range(4):
        rows += list(range(j * 64, (j + 1) * 64)) + list(range((j + 4) * 64, (j + 5) * 64))
    rows += list(range(512, 1024))
    w_out_p = np.ascontiguousarray(w_out[np.array(rows), :])
    qn = np.asarray(inp["q_norm"][0], f32)
    kn = np.asarray(inp["k_norm"][0], f32)
    vecs = np.zeros((128, 8), f32)
    vecs[:, 0] = np.tile(qn, 2)
    vecs[:, 1] = np.tile(qn[_PERM64], 2)
    vecs[:, 2] = np.tile(kn, 2)
    vecs[:, 3] = np.tile(kn[_PERM64], 2)
    vecs[:, 4:6] = np.asarray(inp["cq_norm"][0], f32).reshape(2, 128).T
    vecs[:, 6:8] = np.asarray(inp["ckv_norm"][0], f32).reshape(2, 128).T
    lnp = np.stack([np.broadcast_to(np.asarray(inp[k][0], f32)[None, :], (128, D))
                    for k in ("ln_attn_g", "ln_attn_b", "ln_ffn_g", "ln_ffn_b", "ln_ple_g", "ln_ple_b")]).astype(f32)
    ident = np.eye(128, dtype=f32)
    cbm = np.zeros((128, 3, 128), f32)
    cbm[:, 0, :] = ident
    cbm[0:64, 1, 0:64] = 1.0 / 64
    cbm[64:128, 1, 64:128] = 1.0 / 64
    cbm[:, 2, :] = 1.0 / 256
    return {
        "w_in_p": w_in_p, "w_in_rot": w_in_rot, "w_uq_cat": w_uq_cat,
        "w_ukv": np.ascontiguousarray(np.asarray(inp["w_ukv"][0], f32)), "w_out_p": w_out_p,
        "w_router": np.ascontiguousarray(np.asarray(inp["w_router"][0], f32)),
        **({k: np.ascontiguousarray(np.asarray(inp[k][0], f32)) for k in ("w_gate", "w_up", "w_down") if k in inp}),
        "w_ple_proj": np.ascontiguousarray(np.asarray(inp["w_ple_proj"][0], f32)),
        "w_ple_gate": np.ascontiguousarray(np.asarray(inp["w_ple_gate"][0], f32)),
        "lnp": np.ascontiguousarray(lnp), "vecs": vecs, "tabs": _rope_tables(),
        "ident_f": ident, "cb": cbm.astype(ml_dtypes.bfloat16),
    }


_NC_CACHE = {}


def kernel(**inputs):
    shared = _prep_shared(inputs)
    x = np.asarray(inputs["x"], np.float32)
    p = np.asarray(inputs["p"], np.float32)[0]
    if "nc" not in _NC_CACHE:
        _NC_CACHE["nc"] = build_program()
    nc = _NC_CACHE["nc"]
    in_maps = []
    for c in range(NCORES):
        m = dict(shared)
        m["x"] = np.ascontiguousarray(x[c * SPC:(c + 1) * SPC])
        m["p"] = np.ascontiguousarray(p[c * SPC:(c + 1) * SPC])
        in_maps.append(m)
    res = run_bass_kernel_spmd(nc, in_maps, core_ids=list(range(NCORES)))
    _NC_CACHE["last"] = res
    out = np.concatenate([np.asarray(r["out"]) for r in res.results], axis=0)
    return out.astype(np.float32)
```

```python
import contextlib
import numpy as np
import ml_dtypes
import concourse.bass as bass
import concourse.mybir as mybir
from concourse.bass_utils import run_bass_kernel_spmd

F32 = mybir.dt.float32
BF16 = mybir.dt.bfloat16
U32 = mybir.dt.uint32
AF = mybir.ActivationFunctionType
ALU = mybir.AluOpType
AX = mybir.AxisListType

NCORES = 8
SPC = 2
S = 2048
D = 1024
NT = S // 128
E = 16
CAP = 256
FF = 2816
NFC = FF // 128
PLE = 256
ALPHA = float(2.0 ** 0.25)
LN_EPS = 1e-5
RMS_EPS = 1e-6
NSLOT = 8
DEBUG = False


class Buf:
    __slots__ = ("name", "w", "r", "psum")

    def __init__(self, name="", psum=False):
        self.name = name
        self.w = {}
        self.r = {}
        self.psum = psum


class T:
    _ctr = [0]

    def __init__(self, nc, stack, name, shape, dtype):
        T._ctr[0] += 1
        self.t = stack.enter_context(nc.sbuf_tensor("%s_t%d" % (name, T._ctr[0]), list(shape), dtype))
        self.b = Buf(name)


class Sched:
    ENG = ("pe", "dve", "act", "pool", "sp")

    def __init__(self, nc, stack):
        self.nc = nc
        self.stack = stack
        self.esem = {e: stack.enter_context(nc.semaphore("sem_" + e)) for e in self.ENG}
        self.ecount = {e: 0 for e in self.ENG}
        self.seen = {e: {} for e in self.ENG}
        self.lists = {e: [] for e in self.ENG}
        self.dsems = {}
        self.nops = 0

    def _need(self, e, waits, ev, same_ok):
        sem, val, src = ev
        if same_ok and src == e:
            return
        k = id(sem)
        if self.seen[e].get(k, 0) >= val:
            return
        if k not in waits or waits[k][1] < val:
            waits[k] = (sem, val)

    def _deps(self, e, reads, writes, accw, is_dma):
        waits = {}
        for b in reads:
            for ev in b.w.values():
                self._need(e, waits, ev, (e == "pe") and not is_dma)
            if b.psum:
                for ev in b.r.values():
                    self._need(e, waits, ev, True)
        for b in writes:
            for ev in b.w.values():
                self._need(e, waits, ev, not is_dma)
            for ev in b.r.values():
                self._need(e, waits, ev, not is_dma)
        for b in accw:
            for ev in b.r.values():
                self._need(e, waits, ev, not is_dma)
        return waits

    @staticmethod
    def _put(d, ev):
        k = id(ev[0])
        if k not in d or d[k][1] < ev[1]:
            d[k] = ev

    def _commit(self, e, waits, ev, reads, writes, accw):
        for k, (sem, val) in waits.items():
            self.seen[e][k] = val
        for b in reads:
            self._put(b.r, ev)
        for b in writes:
            b.w = {id(ev[0]): ev}
            b.r = {}
        for b in accw:
            self._put(b.w, ev)

    def op(self, e, fn, reads=(), writes=(), accw=()):
        waits = self._deps(e, reads, writes, accw, False)
        self.ecount[e] += 1
        ev = (self.esem[e], self.ecount[e], e)
        self.lists[e].append((list(waits.values()), fn, self.esem[e], 1))
        self._commit(e, waits, ev, reads, writes, accw)
        self.nops += 1

    def dma(self, q, fn, key, reads=(), writes=(), accw=()):
        waits = self._deps(q, reads, writes, accw, True)
        if key not in self.dsems:
            self.dsems[key] = [self.stack.enter_context(self.nc.semaphore("d%d" % len(self.dsems))), 0]
        ent = self.dsems[key]
        if ent[1] > 0:
            self._need(q, waits, (ent[0], ent[1], "dma"), False)
        ent[1] += 16
        ev = (ent[0], ent[1], "dma")
        self.lists[q].append((list(waits.values()), fn, ent[0], 16))
        self._commit(q, waits, ev, reads, writes, accw)
        self.nops += 1

    def barrier(self):
        for e in self.ENG:
            waits = {}
            for f in self.ENG:
                if f != e and self.ecount[f] > 0:
                    self._need(e, waits, (self.esem[f], self.ecount[f], f), False)
            for ent in self.dsems.values():
                if ent[1] > 0:
                    self._need(e, waits, (ent[0], ent[1], "dma"), False)
            if waits:
                self.lists[e].append((list(waits.values()), None, None, 0))
                for k, (sem, val) in waits.items():
                    self.seen[e][k] = val

    def flush(self):
        self.barrier()
        nc = self.nc
        lists = self.lists
        self.lists = {e: [] for e in self.ENG}

        def body_for(e):
            ops = lists[e]

            def body(eng):
                for waits, fn, sem, inc in ops:
                    for (ws, wv) in waits:
                        eng.wait_ge(ws, wv)
                    if fn is not None:
                        ins = fn(eng)
                        ins.then_inc(sem, inc)
            return body

        with nc.Block() as blk:
            blk.tensor(body_for("pe"))
            blk.vector(body_for("dve"))
            blk.scalar(body_for("act"))
            blk.gpsimd(body_for("pool"))
            blk.sync(body_for("sp"))


class StopBuild(Exception):
    pass


def build_program(stop=None):
    nc = bass.Bass("TRN2", target_bir_lowering=False)
    dumps = {}

    def din(name, shape, dtype):
        return nc.dram_tensor(name, list(shape), dtype, kind="ExternalInput").ap()

    x_d = din("x", [SPC, S, D], F32)
    p_d = din("p", [SPC, S, PLE], F32)
    w_in_d = din("w_in_p", [D, 1312], F32)
    w_rot_d = din("w_in_rot", [D, 672], F32)
    w_uq_d = din("w_uq_cat", [256, 1024], F32)
    w_ukv_d = din("w_ukv", [256, 1024], F32)
    w_out_d = din("w_out_p", [D, D], F32)
    w_r_d = din("w_router", [D, E], F32)
    if stop in (None, "B", "C"):
        wg_d = din("w_gate", [E, D, FF], F32)
        wu_d = din("w_up", [E, D, FF], F32)
        wd_d = din("w_down", [E, FF, D], F32)
    wpp_d = din("w_ple_proj", [PLE, D], F32)
    wpg_d = din("w_ple_gate", [D, D], F32)
    lnp_d = din("lnp", [6, 128, D], F32)
    vecs_d = din("vecs", [128, 8], F32)
    tabs_d = din("tabs", [4, 128, S], F32)
    identf_d = din("ident_f", [128, 128], F32)
    cb_d = din("cb", [128, 3, 128], BF16)
    out_d = nc.dram_tensor("out", [SPC, S, D], F32, kind="ExternalOutput").ap()
    skind = "ExternalOutput" if (DEBUG or stop is not None) else "Internal"
    x1b_d = nc.dram_tensor("x1b_scr", [SPC * S, D], BF16, kind=skind).ap()
    acc_d = nc.dram_tensor("acc_scr", [SPC * S, D], F32, kind=skind).ap()
    wdb_d = nc.dram_tensor("wdb_scr", [E, FF, D], BF16, kind="Internal").ap()
    PRECAST = stop in (None, "B", "C")

    try:
      with contextlib.ExitStack() as top:
        sch = Sched(nc, top)

        def dump(name, t, shape=None):
            ap = t.t[:]
            d = nc.dram_tensor("dbg_" + name, list(ap.shape), ap.dtype, kind="ExternalOutput").ap()
            dumps[name] = d
            sch.dma("sp", lambda q: q.dma_start(out=d, in_=ap), "dump_" + name, reads=[t.b] + (shape or []))

        def stop_here(tag):
            if stop == tag:
                sch.flush()
                raise StopBuild()
        ps = top.enter_context(nc.psum_tensor("ps", [128, 8, 512], F32))
        PB = [Buf("pb%d" % i, psum=True) for i in range(8)]
        bank_ctr = [0]
        pair_ctr = [0]

        def nbank():
            bank_ctr[0] = (bank_ctr[0] + 1) % 8
            return bank_ctr[0]

        def npair():
            pair_ctr[0] = (pair_ctr[0] + 1) % 4
            return 2 * pair_ctr[0]

        def ps2(b0):
            return ps[:, b0:b0 + 2, :].rearrange("p a b -> p (a b)")

        identf = T(nc, top, "identf", [128, 128], F32)
        cb = T(nc, top, "cb", [128, 3, 128], BF16)
        vecs = T(nc, top, "vecs", [128, 8], F32)
        affT = T(nc, top, "affT", [48, S], F32)
        idxT = T(nc, top, "idxT", [128, 2, 48], U32)
        gT = T(nc, top, "gT", [128, 2, 48], F32)
        x1b_buf = Buf("x1b_dram")
        acc_bufs = [Buf("acc_dram%d" % b) for b in range(SPC)]
        wdb_bufs = [Buf("wdb%d" % e) for e in range(E)]

        sch.dma("sp", lambda q: q.dma_start(out=identf.t[:], in_=identf_d), "c0", writes=[identf.b])
        sch.dma("sp", lambda q: q.dma_start(out=cb.t[:], in_=cb_d), "c1", writes=[cb.b])
        sch.dma("sp", lambda q: q.dma_start(out=vecs.t[:], in_=vecs_d), "c2", writes=[vecs.b])
        sch.op("pool", lambda g: g.memset(affT.t[:], 0.0), writes=[affT.b])
        epsr = T(nc, top, "epsr", [128, 4], F32)
        sch.op("pool", lambda g: g.memset(epsr.t[:, 0:1], RMS_EPS), writes=[epsr.b])
        sch.op("pool", lambda g: g.memset(epsr.t[:, 1:2], LN_EPS), accw=[epsr.b])
        sch.op("pool", lambda g: g.memset(epsr.t[:, 2:3], LN_EPS / (ALPHA * ALPHA)), accw=[epsr.b])
        identb = cb.t[:, 0, :]
        blockones = cb.t[:, 1, :]
        ones256 = cb.t[:, 2, :]

        def cast_load(dst_t, dst_ap, src_ap, key):
            sch.dma("pool", lambda q: q.dma_start(out=dst_ap, in_=src_ap), key, writes=[dst_t.b])


        def pipeline(n, stages):
            ns = len(stages)
            for t in range(n + ns - 1):
                for s in reversed(range(ns)):
                    i = t - s
                    if 0 <= i < n:
                        stages[s](i)

        def ln_stats(src_t, stt):
            def f(v):
                v.bn_stats(out=stt.t[:, 0:6], in_=src_t.t[:, 0:512])
                return v.bn_stats(out=stt.t[:, 6:12], in_=src_t.t[:, 512:1024])
            sch.op("dve", f, reads=[src_t.b], writes=[stt.b])
            sch.op("dve", lambda v: v.bn_aggr(out=stt.t[:, 12:14], in_=stt.t[:, 0:12]), reads=[stt.b], accw=[stt.b])
            sch.op("act", lambda a: a.activation(out=stt.t[:, 14:15], in_=stt.t[:, 13:14], func=AF.Sqrt, bias=epsr.t[:, 1:2], scale=1.0),
                   reads=[stt.b, epsr.b], accw=[stt.b])
            sch.op("dve", lambda v: v.reciprocal(out=stt.t[:, 14:15], in_=stt.t[:, 14:15]), reads=[stt.b], accw=[stt.b])
            sch.op("dve", lambda v: v.scalar_tensor_tensor(out=stt.t[:, 15:16], in0=stt.t[:, 12:13], scalar=-1.0, in1=stt.t[:, 14:15],
                                                           op0=ALU.mult, op1=ALU.mult), reads=[stt.b], accw=[stt.b])

        def ln_apply(src_t, stt, g_t, b_t, tmp_t, dst_t, g_eng="pool", b_eng="pool"):
            sch.op("act", lambda a: a.activation(out=tmp_t.t[:], in_=src_t.t[:], func=AF.Identity, bias=stt.t[:, 15:16], scale=stt.t[:, 14:15]),
                   reads=[src_t.b, stt.b], writes=[tmp_t.b])
            sch.op(g_eng, lambda g: g.tensor_tensor(out=tmp_t.t[:], in0=tmp_t.t[:], in1=g_t.t[:], op=ALU.mult),
                   reads=[tmp_t.b, g_t.b], writes=[tmp_t.b])
            sch.op(b_eng, lambda g: g.tensor_tensor(out=dst_t.t[:], in0=tmp_t.t[:], in1=b_t.t[:], op=ALU.add),
                   reads=[tmp_t.b, b_t.b], writes=[dst_t.b])

        for b in range(SPC):
            with contextlib.ExitStack() as sa:
                big = T(nc, sa, "big", [128, 8, S], BF16)
                bigb = [[Buf("big%d_%d" % (k, q)) for q in range(4)] for k in range(8)]
                bigall = [bb for row in bigb for bb in row]
                with contextlib.ExitStack() as sp_:
                    qg = [T(nc, sp_, "qg%d" % c, [128, S], BF16) for c in range(4)]
                    kz = [T(nc, sp_, "kz%d" % i, [128, S], BF16) for i in range(2)]
                    VgA = T(nc, sp_, "VgA", [128, NT, 2, 128], BF16)
                    cqn = T(nc, sp_, "cqn", [128, 2, S], BF16)
                    ckvn = T(nc, sp_, "ckvn", [128, 2, S], BF16)
                    kpe = T(nc, sp_, "kpe", [128, S], BF16)
                    cosM = T(nc, sp_, "cosM", [128, S], F32)
                    sinM = T(nc, sp_, "sinM", [128, S], F32)
                    w_uq = T(nc, sp_, "w_uq", [128, 2, 1024], BF16)
                    w_ukv = T(nc, sp_, "w_ukv", [128, 2, 1024], BF16)
                    with contextlib.ExitStack() as s0:
                        w_in = T(nc, s0, "w_in", [128, 8, 1312], BF16)
                        w_rot = T(nc, s0, "w_rot", [128, 8, 672], BF16)
                        xin = [T(nc, s0, "xin%d" % i, [128, D], F32) for i in range(2)]
                        cosG = T(nc, s0, "cosG", [128, S], F32)
                        sinG = T(nc, s0, "sinG", [128, S], F32)
                        sqb = [T(nc, s0, "sqb%d" % i, [128, 512], BF16) for i in range(4)]
                        rstd = [T(nc, s0, "rstd%d" % i, [128, 512], F32) for i in range(2)]
                        t1 = [T(nc, s0, "t1_%d" % i, [128, 512], F32) for i in range(2)]
                        t2 = [T(nc, s0, "t2_%d" % i, [128, 512], F32) for i in range(2)]
                        t3 = [T(nc, s0, "t3_%d" % i, [128, 512], F32) for i in range(2)]

                        w_in.pieces = [Buf("w_in_p%d" % i) for i in range(6)]
                        w_rot.pieces = [Buf("w_rot_p%d" % i) for i in range(6)]
                        w_in_v = w_in_d.rearrange("(k p) n -> p k n", p=128)
                        w_rot_v = w_rot_d.rearrange("(k p) n -> p k n", p=128)
                        for pc in range(6):
                            for (wt_, wv_, hi_, tag) in ((w_in, w_in_v, 1312, "wi"), (w_rot, w_rot_v, 672, "wr")):
                                c0_ = pc * 128
                                c1_ = c0_ + 128 if pc < 5 else hi_
                                sch.dma("pool", lambda q, wt_=wt_, wv_=wv_, c0_=c0_, c1_=c1_: q.dma_start(out=wt_.t[:, :, c0_:c1_], in_=wv_[:, :, c0_:c1_]),
                                        "%s%d" % (tag, pc), writes=[wt_.pieces[pc]])
                        cast_load(w_uq, w_uq.t[:], w_uq_d.rearrange("(k p) n -> p k n", p=128), "w2")
                        cast_load(w_ukv, w_ukv.t[:], w_ukv_d.rearrange("(k p) n -> p k n", p=128), "w3b")
                        for tt, ti in ((cosG, 0), (sinG, 1), (cosM, 2), (sinM, 3)):
                            sch.dma("sp", lambda q, tt=tt, ti=ti: q.dma_start(out=tt.t[:], in_=tabs_d[ti]),
                                    "tab%d" % ti, writes=[tt.b])
                        sch.op("pool", lambda g: g.memset(VgA.t[:], 1.0), writes=[VgA.b])
                        for _z in range(2):
                            sch.op("pool", lambda g, _z=_z: g.memset(kz[_z].t[:], 0.0), writes=[kz[_z].b])

                        def xload(i):
                            xi = xin[i % 2]
                            sch.dma("sp", lambda q, xi=xi, i=i: q.dma_start(out=xi.t[:], in_=x_d[b, i * 128:(i + 1) * 128, :]),
                                    "xin%d" % (i % 2), writes=[xi.b])
                            for hb in range(2):
                                bk = nbank()

                                def tr(pe, xi=xi, hb=hb, bk=bk):
                                    for k4 in range(4):
                                        ins = pe.transpose(out=ps[:, bk, k4 * 128:(k4 + 1) * 128],
                                                           in_=xi.t[:, (hb * 4 + k4) * 128:(hb * 4 + k4 + 1) * 128],
                                                           identity=identf.t[:])
                                    return ins
                                sch.op("pe", tr, reads=[xi.b, identf.b], writes=[PB[bk]])
                                dst = big.t[:, hb * 4:(hb + 1) * 4, i * 128:(i + 1) * 128]
                                src = ps[:, bk, :].rearrange("p (k c) -> p k c", k=4)
                                if hb == 0:
                                    sch.op("act", lambda a, dst=dst, src=src: a.copy(out=dst, in_=src),
                                           reads=[PB[bk]], accw=[bigb[k_][i // 4] for k_ in range(hb * 4, (hb + 1) * 4)])
                                else:
                                    sch.op("dve", lambda v, dst=dst, src=src: v.tensor_copy(out=dst, in_=src),
                                           reads=[PB[bk]], accw=[bigb[k_][i // 4] for k_ in range(hb * 4, (hb + 1) * 4)])

                        def mm_fm(bk, w_t, col0, M, nb, prow=0):
                            def f(pe):
                                for k in range(8):
                                    ins = pe.matmul(ps[prow:prow + M, bk, :], lhsT=w_t.t[:, k, col0:col0 + M],
                                                    rhs=big.t[:, k, nb * 512:(nb + 1) * 512],
                                                    start=(k == 0), stop=(k == 7))
                                return ins
                            sch.op("pe", f, reads=[w_t.pieces[min(col0 // 128, 5)]] + [bigb[k_][nb] for k_ in range(8)], writes=[PB[bk]])

                        it = 0
                        for c in range(5):
                            gi = 0 if c < 4 else 2
                            dst_t = qg[c] if c < 4 else None
                            for nb in range(4):
                                if c == 0:
                                    for i_ in range(nb * 4, nb * 4 + 4):
                                        xload(i_)
                                j = it % 2
                                it += 1
                                bA, bB, bC = nbank(), nbank(), nbank()
                                blk = slice(nb * 512, (nb + 1) * 512)
                                mm_fm(bA, w_in, c * 128, 128, nb)
                                mm_fm(bB, w_rot, c * 128, 128, nb)
                                sq = sqb[it % 4]
                                sch.op("act", lambda a, sq=sq, bA=bA: a.activation(out=sq.t[:], in_=ps[:, bA, :], func=AF.Square),
                                       reads=[PB[bA]], writes=[sq.b])
                                sch.op("pe", lambda pe, sq=sq, bC=bC: pe.matmul(ps[:, bC, :], lhsT=blockones, rhs=sq.t[:], start=True, stop=True),
                                       reads=[sq.b, cb.b], writes=[PB[bC]])
                                sch.op("act", lambda a, j=j, bC=bC: a.activation(out=rstd[j].t[:], in_=ps[:, bC, :], func=AF.Sqrt, bias=epsr.t[:, 0:1], scale=1.0),
                                       reads=[PB[bC], epsr.b], writes=[rstd[j].b])
                                sch.op("dve", lambda v, j=j: v.reciprocal(out=rstd[j].t[:], in_=rstd[j].t[:]),
                                       reads=[rstd[j].b], writes=[rstd[j].b])
                                sch.op("dve", lambda v, j=j, bA=bA, gi=gi, blk=blk: v.scalar_tensor_tensor(
                                    out=t1[j].t[:], in0=ps[:, bA, :], scalar=vecs.t[:, gi:gi + 1], in1=cosG.t[:, blk],
                                    op0=ALU.mult, op1=ALU.mult), reads=[PB[bA], vecs.b, cosG.b], writes=[t1[j].b])
                                sch.op("dve", lambda v, j=j, bB=bB, gi=gi, blk=blk: v.scalar_tensor_tensor(
                                    out=t2[j].t[:], in0=ps[:, bB, :], scalar=vecs.t[:, gi + 1:gi + 2], in1=sinG.t[:, blk],
                                    op0=ALU.mult, op1=ALU.mult), reads=[PB[bB], vecs.b, sinG.b], writes=[t2[j].b])
                                sch.op("pool", lambda g, j=j: g.tensor_tensor(out=t3[j].t[:], in0=t1[j].t[:], in1=t2[j].t[:], op=ALU.add),
                                       reads=[t1[j].b, t2[j].b], writes=[t3[j].b])
                                if dst_t is not None:
                                    sch.op("pool", lambda g, j=j, dst_t=dst_t, blk=blk: g.tensor_tensor(
                                        out=dst_t.t[:, blk], in0=t3[j].t[:], in1=rstd[j].t[:], op=ALU.mult),
                                        reads=[t3[j].b, rstd[j].b], accw=[dst_t.b])
                                else:
                                    for _z, rows in ((0, slice(0, 64)), (1, slice(64, 128))):
                                        sch.op("pool", lambda g, j=j, _z=_z, rows=rows, blk=blk: g.tensor_tensor(
                                            out=kz[_z].t[rows, blk], in0=t3[j].t[rows, :], in1=rstd[j].t[rows, :], op=ALU.mult),
                                            reads=[t3[j].b, rstd[j].b], accw=[kz[_z].b])

                        if b == 0 and stop == "A0b":
                            dump("qg0", qg[0])
                            stop_here("A0b")
                        for (dst_t, col0, gi) in ((cqn, 768, 4), (ckvn, 1024, 6)):
                            for nb in range(4):
                                j = it % 2
                                it += 1
                                blk = slice(nb * 512, (nb + 1) * 512)
                                bA0, bA1, bC = nbank(), nbank(), nbank()
                                mm_fm(bA0, w_in, col0, 128, nb)
                                mm_fm(bA1, w_in, col0 + 128, 128, nb)
                                s0_, s1_ = sqb[(2 * it) % 4], sqb[(2 * it + 1) % 4]
                                sch.op("act", lambda a, s0_=s0_, bA0=bA0: a.activation(out=s0_.t[:], in_=ps[:, bA0, :], func=AF.Square),
                                       reads=[PB[bA0]], writes=[s0_.b])
                                sch.op("act", lambda a, s1_=s1_, bA1=bA1: a.activation(out=s1_.t[:], in_=ps[:, bA1, :], func=AF.Square),
                                       reads=[PB[bA1]], writes=[s1_.b])

                                def msf(pe, s0_=s0_, s1_=s1_, bC=bC):
                                    pe.matmul(ps[:, bC, :], lhsT=ones256, rhs=s0_.t[:], start=True, stop=False)
                                    return pe.matmul(ps[:, bC, :], lhsT=ones256, rhs=s1_.t[:], start=False, stop=True)
                                sch.op("pe", msf, reads=[s0_.b, s1_.b, cb.b], writes=[PB[bC]])
                                sch.op("act", lambda a, j=j, bC=bC: a.activation(out=rstd[j].t[:], in_=ps[:, bC, :], func=AF.Sqrt, bias=epsr.t[:, 0:1], scale=1.0),
                                       reads=[PB[bC], epsr.b], writes=[rstd[j].b])
                                sch.op("dve", lambda v, j=j: v.reciprocal(out=rstd[j].t[:], in_=rstd[j].t[:]),
                                       reads=[rstd[j].b], writes=[rstd[j].b])
                                for kc, bA in ((0, bA0), (1, bA1)):
                                    sch.op("dve", lambda v, j=j, bA=bA, kc=kc, gi=gi, blk=blk, dst_t=dst_t: v.scalar_tensor_tensor(
                                        out=dst_t.t[:, kc, blk], in0=ps[:, bA, :], scalar=vecs.t[:, gi + kc:gi + kc + 1], in1=rstd[j].t[:],
                                        op0=ALU.mult, op1=ALU.mult), reads=[PB[bA], vecs.b, rstd[j].b], accw=[dst_t.b])

                        if b == 0 and stop == "A0c":
                            dump("cqn", cqn)
                            stop_here("A0c")
                        for nb in range(4):
                            j = it % 2
                            it += 1
                            blk = slice(nb * 512, (nb + 1) * 512)
                            bA, bB = nbank(), nbank()
                            mm_fm(bA, w_in, 1280, 32, nb, prow=64)
                            mm_fm(bB, w_rot, 640, 32, nb, prow=64)
                            sch.op("dve", lambda v, j=j, bA=bA, blk=blk: v.tensor_tensor(out=t1[j].t[64:96, :], in0=ps[64:96, bA, :], in1=cosM.t[64:96, blk], op=ALU.mult),
                                   reads=[PB[bA], cosM.b], writes=[t1[j].b])
                            sch.op("dve", lambda v, j=j, bB=bB, blk=blk: v.tensor_tensor(out=t2[j].t[64:96, :], in0=ps[64:96, bB, :], in1=sinM.t[64:96, blk], op=ALU.mult),
                                   reads=[PB[bB], sinM.b], writes=[t2[j].b])
                            sch.op("pool", lambda g, j=j, blk=blk: g.tensor_tensor(out=kpe.t[64:96, blk], in0=t1[j].t[64:96, :], in1=t2[j].t[64:96, :], op=ALU.add),
                                   reads=[t1[j].b, t2[j].b], accw=[kpe.b])

                        if b == 0 and stop == "A0d":
                            dump("kpe", kpe)
                            stop_here("A0d")
                        for i0 in range(0, NT, 4):
                            bk = nbank()

                            def vf(pe, i0=i0, bk=bk):
                                for jj in range(4):
                                    i = i0 + jj
                                    for k in range(8):
                                        ins = pe.matmul(ps[:, bk, jj * 128:(jj + 1) * 128], lhsT=big.t[:, k, i * 128:(i + 1) * 128],
                                                        rhs=w_in.t[:, k, 640:768], start=(k == 0), stop=(k == 7))
                                return ins
                            sch.op("pe", vf, reads=[w_in.pieces[5]] + [bigb[k_][i0 // 4] for k_ in range(8)], writes=[PB[bk]])
                            src = ps[:, bk, :].rearrange("p (i d) -> p i d", i=4)
                            sch.op("act", lambda a, i0=i0, src=src: a.copy(out=VgA.t[:, i0:i0 + 4, 0, 0:64], in_=src[:, :, 0:64]),
                                   reads=[PB[bk]], accw=[VgA.b])
                            sch.op("dve", lambda v, i0=i0, src=src: v.tensor_copy(out=VgA.t[:, i0:i0 + 4, 1, 64:128], in_=src[:, :, 64:128]),
                                   reads=[PB[bk]], accw=[VgA.b])
                        if b == 0 and stop == "A0":
                            for _c in range(4):
                                dump("qg%d" % _c, qg[_c])
                            dump("kg0", kz[0]); dump("kg1", kz[1]); dump("VgA", VgA); dump("cqn", cqn); dump("ckvn", ckvn); dump("kpe", kpe); dump("big", big, bigall)
                            stop_here("A0")
                        sch.flush()

                    w_out = T(nc, sp_, "w_out", [128, 8, D], BF16)
                    with contextlib.ExitStack() as s1:
                        mq = [T(nc, s1, "mq%d" % i, [128, S], BF16) for i in range(2)]
                        mk = [T(nc, s1, "mk%d" % i, [128, S], BF16) for i in range(2)]
                        mV = [T(nc, s1, "mV%d" % i, [128, NT, 128], BF16) for i in range(2)]
                        PT = [T(nc, s1, "PT%d" % i, [128, 1024], BF16) for i in range(3)]
                        acsb = [T(nc, s1, "acsb%d" % i, [128, 1024], F32) for i in range(2)]
                        dns = [T(nc, s1, "dns%d" % i, [128, 1024], F32) for i in range(2)]
                        scr = [T(nc, s1, "scr%d" % i, [128, 1024], F32) for i in range(2)]
                        nrm = [0]
                        m1 = [T(nc, s1, "m1_%d" % i, [128, 512], F32) for i in range(2)]
                        m2 = [T(nc, s1, "m2_%d" % i, [128, 512], F32) for i in range(2)]

                        cast_load(w_out, w_out.t[:], w_out_d.rearrange("(k p) n -> p k n", p=128), "w0")
                        for i in range(2):
                            sch.op("pool", lambda g, i=i: g.memset(mV[i].t[:], 1.0), writes=[mV[i].b])
                            sch.op("pool", lambda g, i=i: g.tensor_copy(out=mk[i].t[64:96, :], in_=kpe.t[64:96, :]),
                                   reads=[kpe.b], accw=[mk[i].b])

                        mctr = [0]
                        cur_step = [0]
                        deferred = {}

                        def defer(k, fn):
                            deferred.setdefault(cur_step[0] + k, []).append(fn)


                        def mla_prep_tasks(h):
                            bf = h % 2
                            tasks = []

                            def qk_task(nb):
                                blk = slice(nb * 512, (nb + 1) * 512)
                                j = mctr[0] % 2
                                mctr[0] += 1
                                bA, bB = 6, 7

                                def qf(pe):
                                    for kc in range(2):
                                        pe.matmul(ps[:, bA, :], lhsT=w_uq.t[:, kc, h * 128:(h + 1) * 128], rhs=cqn.t[:, kc, blk],
                                                  start=(kc == 0), stop=(kc == 1))
                                    for kc in range(2):
                                        ins = pe.matmul(ps[:, bB, :], lhsT=w_ukv.t[:, kc, h * 128:(h + 1) * 128], rhs=ckvn.t[:, kc, blk],
                                                        start=(kc == 0), stop=(kc == 1))
                                    return ins
                                sch.op("pe", qf, reads=[w_uq.b, w_ukv.b, cqn.b, ckvn.b], writes=[PB[bA], PB[bB]])

                                def evac():
                                    sch.op("dve", lambda v: v.tensor_copy(out=mq[bf].t[0:64, blk], in_=ps[0:64, bA, :]),
                                           reads=[PB[bA]], accw=[mq[bf].b])
                                    sch.op("dve", lambda v: v.tensor_tensor(out=m1[j].t[64:96, :], in0=ps[64:96, bA, :], in1=cosM.t[64:96, blk], op=ALU.mult),
                                           reads=[PB[bA], cosM.b], writes=[m1[j].b])
                                    sch.op("dve", lambda v: v.tensor_tensor(out=m2[j].t[96:128, :], in0=ps[96:128, bA, :], in1=sinM.t[96:128, blk], op=ALU.mult),
                                           reads=[PB[bA], sinM.b], writes=[m2[j].b])
                                    sch.op("pool", lambda g: g.tensor_copy(out=m2[j].t[64:96, :], in_=m2[j].t[96:128, :]),
                                           reads=[m2[j].b], accw=[m2[j].b])
                                    sch.op("pool", lambda g: g.tensor_tensor(out=mq[bf].t[64:96, blk], in0=m1[j].t[64:96, :], in1=m2[j].t[64:96, :], op=ALU.add),
                                           reads=[m1[j].b, m2[j].b], accw=[mq[bf].b])
                                    sch.op("dve", lambda v: v.tensor_copy(out=mk[bf].t[0:64, blk], in_=ps[0:64, bB, :]),
                                           reads=[PB[bB]], accw=[mk[bf].b])
                                defer(1, evac)

                            def v_task():
                                b0 = 6

                                def vf(pe):
                                    pv = ps2(b0)
                                    for i in range(NT):
                                        for kc in range(2):
                                            ins = pe.matmul(pv[:, i * 64:(i + 1) * 64], lhsT=ckvn.t[:, kc, i * 128:(i + 1) * 128],
                                                            rhs=w_ukv.t[:, kc, h * 128 + 64:h * 128 + 128], start=(kc == 0), stop=(kc == 1))
                                    return ins
                                sch.op("pe", vf, reads=[w_ukv.b, ckvn.b], writes=[PB[b0], PB[b0 + 1]])
                                off = 0 if h % 2 == 0 else 64
                                defer(1, lambda: sch.op("dve", lambda v: v.tensor_copy(out=mV[bf].t[:, :, off:off + 64],
                                                                                      in_=ps2(b0).rearrange("p (i d) -> p i d", d=64)),
                                                        reads=[PB[b0], PB[b0 + 1]], accw=[mV[bf].b]))
                            for nb in range(4):
                                tasks.append(lambda nb=nb: qk_task(nb))
                            tasks.append(v_task)
                            return tasks

                        heads = []
                        for jc in range(4):
                            heads.append(dict(q=qg[jc].t[:, :], k=kz[0].t[:, :], qb=qg[jc].b, kb=kz[0].b,
                                              va=(lambda i: VgA.t[:, i, 0, :]), vb=VgA.b, chunk=jc, odd=False, scale=64 ** -0.5, mla=None))
                            heads.append(dict(q=qg[jc].t[:, :], k=kz[1].t[:, :], qb=qg[jc].b, kb=kz[1].b,
                                              va=(lambda i: VgA.t[:, i, 1, :]), vb=VgA.b, chunk=jc, odd=True, scale=64 ** -0.5, mla=None))
                        for h in range(8):
                            bf = h % 2
                            heads.append(dict(q=mq[bf].t[0:96, :], k=mk[bf].t[0:96, :], qb=mq[bf].b, kb=mk[bf].b,
                                              va=(lambda i, bf=bf: mV[bf].t[:, i, :]), vb=mV[bf].b, chunk=4 + h // 2, odd=(h % 2 == 1),
                                              scale=96 ** -0.5, mla=h))
                        steps = []
                        for hi, hd in enumerate(heads):
                            for half in range(2):
                                for i in range(NT):
                                    steps.append((hi, hd, half, i))

                        sctr = [0]
                        accs = {}

                        def emit_qk(st):
                            hi, hd, half, i = st
                            if half == 0 and i == 0 and hd["mla"] is not None and hd["mla"] == 0:
                                pass
                            sb = 0 if (sctr[0] % 2 == 0) else 2
                            pt = PT[sctr[0] % 3]
                            sctr[0] += 1

                            def f(pe, hd=hd, half=half, i=i, sb=sb):
                                for c in range(2):
                                    ins = pe.matmul(ps[:, sb + c, :], lhsT=hd["k"][:, i * 128:(i + 1) * 128],
                                                    rhs=hd["q"][:, half * 1024 + c * 512: half * 1024 + (c + 1) * 512],
                                                    start=True, stop=True)
                                return ins
                            sch.op("pe", f, reads=[hd["qb"], hd["kb"]], writes=[PB[sb], PB[sb + 1]])
                            sch.op("act", lambda a, sb=sb, pt=pt, hd=hd: a.activation(out=pt.t[:], in_=ps2(sb), func=AF.Exp, scale=float(hd["scale"])),
                                   reads=[PB[sb], PB[sb + 1]], writes=[pt.b])
                            return pt

                        def emit_pv(st, pt):
                            hi, hd, half, i = st
                            key = (hi, half)
                            if key not in accs:
                                if hd["mla"] is None and hi < 7:
                                    accs[key] = 4 if (len(accs) % 2 == 0) else 6
                                else:
                                    accs[key] = 4
                            ab = accs[key]

                            def f(pe, hd=hd, i=i, ab=ab, pt=pt):
                                for c in range(2):
                                    ins = pe.matmul(ps[:, ab + c, :], lhsT=hd["va"](i), rhs=pt.t[:, c * 512:(c + 1) * 512],
                                                    start=(i == 0), stop=(i == NT - 1))
                                return ins
                            sch.op("pe", f, reads=[hd["vb"], pt.b], writes=[PB[ab], PB[ab + 1]] if i == 0 else (), accw=() if i == 0 else [PB[ab], PB[ab + 1]])
                            if i == NT - 1:
                                num = slice(64, 128) if hd["odd"] else slice(0, 64)
                                den = slice(0, 64) if hd["odd"] else slice(64, 128)
                                nrm[0] += 1
                                ac = acsb[nrm[0] % 2]
                                dn = dns[nrm[0] % 2]
                                sc_ = scr[nrm[0] % 2]
                                acc2 = ps2(ab)
                                sch.op("dve", lambda v, ac=ac, acc2=acc2: v.tensor_copy(out=ac.t[:], in_=acc2),
                                       reads=[PB[ab], PB[ab + 1]], writes=[ac.b])
                                sch.op("pool", lambda g, ac=ac, dn=dn, num=num, den=den: g.tensor_copy(out=dn.t[num, 0:512], in_=ac.t[den, 512:1024]),
                                       reads=[ac.b], accw=[dn.b])
                                sch.op("pool", lambda g, ac=ac, dn=dn, den=den: g.tensor_copy(out=dn.t[den, 0:512], in_=ac.t[den, 0:512]),
                                       reads=[ac.b], accw=[dn.b])
                                ch = hd["chunk"]
                                c0 = half * 1024

                                def nrm_a(dn=dn, sc_=sc_, num=num, den=den):
                                    sch.op("dve", lambda v: v.reciprocal(out=sc_.t[:, 0:512], in_=dn.t[:, 0:512]),
                                           reads=[dn.b], writes=[sc_.b])
                                    sch.op("pool", lambda g: g.tensor_copy(out=sc_.t[num, 512:1024], in_=sc_.t[den, 0:512]),
                                           reads=[sc_.b], accw=[sc_.b])

                                def nrm_b(ac=ac, sc_=sc_, num=num, ch=ch, c0=c0):
                                    sch.op("dve", lambda v: v.tensor_tensor(
                                        out=big.t[num, ch, c0 + 512:c0 + 1024], in0=ac.t[num, 512:1024], in1=sc_.t[num, 0:512], op=ALU.mult),
                                        reads=[ac.b, sc_.b], accw=[bigb[ch][c0 // 512 + 1]])
                                    sch.op("dve", lambda v: v.tensor_tensor(
                                        out=big.t[num, ch, c0:c0 + 512], in0=ac.t[num, 0:512], in1=sc_.t[num, 512:1024], op=ALU.mult),
                                        reads=[ac.b, sc_.b], accw=[bigb[ch][c0 // 512]])
                                defer(3, nrm_a)
                                defer(8, nrm_b)

                        pvq = []
                        pending = []
                        for si, st in enumerate(steps):
                            hi, hd, half, i = st
                            cur_step[0] = si
                            for fn in deferred.pop(si, []):
                                fn()
                            if half == 0 and i == 0:
                                nxt = hi + 1
                                if nxt < len(heads) and heads[nxt]["mla"] is not None:
                                    pending = mla_prep_tasks(heads[nxt]["mla"])
                            if pending and (i % 4 == 2):
                                pending.pop(0)()
                            if PRECAST and half == 0 and i == 8 and hi % 2 == 0:
                                e_ = b * (E // SPC) + hi // 2
                                sch.dma("pool", lambda q, e_=e_: q.dma_start(out=wdb_d[e_], in_=wd_d[e_]), "precast%d" % (e_ % 2),
                                        writes=[wdb_bufs[e_]])
                            pt = emit_qk(st)
                            pvq.append((st, pt))
                            if len(pvq) > 2:
                                emit_pv(*pvq.pop(0))
                        cur_step[0] = len(steps)
                        while pvq:
                            emit_pv(*pvq.pop(0))
                        for k in sorted(deferred):
                            for fn in deferred[k]:
                                fn()
                        deferred.clear()
                        if b == 0 and stop == "A1":
                            dump("big", big, bigall); dump("mq1", mq[1]); dump("mk1", mk[1]); dump("mV1", mV[1])
                            stop_here("A1")
                        sch.flush()

                    with contextlib.ExitStack() as s2:
                        def rt2(name, shape, dtype, depth):
                            return [T(nc, s2, "%s%d" % (name, i), shape, dtype) for i in range(depth)]

                        def R2(lst, n):
                            return lst[n % len(lst)]
                        w_r = T(nc, s2, "w_r", [128, 8, E], F32)
                        g1 = T(nc, s2, "g1", [128, D], F32)
                        b1 = T(nc, s2, "b1", [128, D], F32)
                        xin = rt2("xin2_", [128, D], F32, 3)
                        rr = rt2("rr", [128, D], F32, 4)
                        st_ = rt2("st", [128, 16], F32, 4)
                        xn = rt2("xn", [128, D], F32, 2)
                        x1 = rt2("x1_", [128, D], F32, 2)
                        accx = rt2("accx", [128, D], F32, 2)
                        x1b = rt2("x1b", [128, D], BF16, 2)
                        x1T = rt2("x1T", [128, 8, 128], F32, 2)
                        sm = rt2("sm", [128, 8], F32, 3)
                        ex = rt2("ex", [128, E], F32, 2)
                        aff = rt2("aff", [128, E], F32, 2)

                        sch.dma("sp", lambda q: q.dma_start(out=w_r.t[:], in_=w_r_d.rearrange("(k p) n -> p k n", p=128)), "w3", writes=[w_r.b])
                        sch.dma("sp", lambda q: q.dma_start(out=g1.t[:], in_=lnp_d[0]), "ln0", writes=[g1.b])
                        sch.dma("sp", lambda q: q.dma_start(out=b1.t[:], in_=lnp_d[1]), "ln1", writes=[b1.b])

                        def sa(src_t, stt):
                            def f(v):
                                v.bn_stats(out=stt.t[:, 0:6], in_=src_t.t[:, 0:512])
                                return v.bn_stats(out=stt.t[:, 6:12], in_=src_t.t[:, 512:1024])
                            sch.op("dve", f, reads=[src_t.b], writes=[stt.b])
                            sch.op("dve", lambda v: v.bn_aggr(out=stt.t[:, 12:14], in_=stt.t[:, 0:12]), reads=[stt.b], accw=[stt.b])

                        def a0(i):
                            tok = slice(i * 128, (i + 1) * 128)
                            xi = R2(xin, i)
                            sch.dma("sp", lambda q: q.dma_start(out=xi.t[:], in_=x_d[b, tok, :]), "xin%d" % (i % len(xin)), writes=[xi.b])
                            b0 = 0 if i % 2 == 0 else 2

                            def of(pe):
                                for hf in range(2):
                                    for k in range(8):
                                        ins = pe.matmul(ps[:, b0 + hf, :], lhsT=big.t[:, k, tok], rhs=w_out.t[:, k, hf * 512:(hf + 1) * 512],
                                                        start=(k == 0), stop=(k == 7))
                                return ins
                            sch.op("pe", of, reads=[w_out.b] + [bigb[k_][i // 4] for k_ in range(8)], writes=[PB[b0], PB[b0 + 1]])

                        def a1(i):
                            xi, r_, stt = R2(xin, i), R2(rr, i), R2(st_, i)
                            b0 = 0 if i % 2 == 0 else 2
                            sch.op("dve", lambda v: v.scalar_tensor_tensor(out=r_.t[:], in0=xi.t[:], scalar=ALPHA, in1=ps2(b0),
                                                                           op0=ALU.mult, op1=ALU.add),
                                   reads=[xi.b, PB[b0], PB[b0 + 1]], writes=[r_.b])
                            sa(r_, stt)

                        def a2_(i):
                            stt = R2(st_, i)
                            sch.op("act", lambda a: a.activation(out=stt.t[:, 14:15], in_=stt.t[:, 13:14], func=AF.Sqrt, bias=epsr.t[:, 1:2], scale=1.0),
                                   reads=[stt.b, epsr.b], accw=[stt.b])

                        def a3(i):
                            stt = R2(st_, i)
                            sch.op("dve", lambda v: v.reciprocal(out=stt.t[:, 14:15], in_=stt.t[:, 14:15]), reads=[stt.b], accw=[stt.b])
                            sch.op("dve", lambda v: v.scalar_tensor_tensor(out=stt.t[:, 15:16], in0=stt.t[:, 12:13], scalar=-1.0, in1=stt.t[:, 14:15],
                                                                           op0=ALU.mult, op1=ALU.mult), reads=[stt.b], accw=[stt.b])

                        def a4(i):
                            r_, stt, xn_ = R2(rr, i), R2(st_, i), R2(xn, i)
                            sch.op("act", lambda a: a.activation(out=xn_.t[:], in_=r_.t[:], func=AF.Identity, bias=stt.t[:, 15:16], scale=stt.t[:, 14:15]),
                                   reads=[r_.b, stt.b], writes=[xn_.b])

                        def a5(i):
                            xn_, x1_ = R2(xn, i), R2(x1, i)
                            sch.op("pool", lambda g: g.tensor_tensor(out=xn_.t[:], in0=xn_.t[:], in1=g1.t[:], op=ALU.mult), reads=[xn_.b, g1.b], writes=[xn_.b])
                            sch.op("pool", lambda g: g.tensor_tensor(out=x1_.t[:], in0=xn_.t[:], in1=b1.t[:], op=ALU.add), reads=[xn_.b, b1.b], writes=[x1_.b])

                        def a6(i):
                            x1_, ax, xb = R2(x1, i), R2(accx, i), R2(x1b, i)
                            row0 = b * S + i * 128
                            sch.op("act", lambda a: a.activation(out=ax.t[:], in_=x1_.t[:], func=AF.Copy, scale=ALPHA), reads=[x1_.b], writes=[ax.b])
                            sch.dma("sp", lambda q: q.dma_start(out=acc_d[row0:row0 + 128, :], in_=ax.t[:]), "accst%d" % (i % 2),
                                    reads=[ax.b], accw=[acc_bufs[b]])
                            sch.op("act", lambda a: a.copy(out=xb.t[:], in_=x1_.t[:]), reads=[x1_.b], writes=[xb.b])
                            sch.dma("sp", lambda q: q.dma_start(out=x1b_d[row0:row0 + 128, :], in_=xb.t[:]), "x1bst%d" % (i % 2),
                                    reads=[xb.b], accw=[x1b_buf])

                            def trf(pe):
                                pv = ps2(4)
                                for k in range(8):
                                    ins = pe.transpose(out=pv[:, k * 128:(k + 1) * 128], in_=x1_.t[:, k * 128:(k + 1) * 128], identity=identf.t[:])
                                return ins
                            sch.op("pe", trf, reads=[x1_.b, identf.b], writes=[PB[4], PB[5]])

                        def a7(i):
                            xt_ = R2(x1T, i)
                            sch.op("dve", lambda v: v.tensor_copy(out=xt_.t[:].rearrange("p k c -> p (k c)"), in_=ps2(4)),
                                   reads=[PB[4], PB[5]], writes=[xt_.b])

                        def a8(i):
                            xt_ = R2(x1T, i)
                            bl = 6 + i % 2

                            def lf(pe):
                                for k in range(8):
                                    ins = pe.matmul(ps[:, bl, 0:E], lhsT=xt_.t[:, k, :], rhs=w_r.t[:, k, :], start=(k == 0), stop=(k == 7))
                                return ins
                            sch.op("pe", lf, reads=[xt_.b, w_r.b], writes=[PB[bl]])

                        def a9(i):
                            s_ = R2(sm, i)
                            bl = 6 + i % 2
                            sch.op("dve", lambda v: v.reduce_max(out=s_.t[:, 0:1], in_=ps[:, bl, 0:E], axis=AX.X), reads=[PB[bl]], writes=[s_.b])
                            sch.op("dve", lambda v: v.tensor_scalar_mul(out=s_.t[:, 1:2], in0=s_.t[:, 0:1], scalar1=-1.0), reads=[s_.b], accw=[s_.b])

                        def a10(i):
                            s_, e_ = R2(sm, i), R2(ex, i)
                            bl = 6 + i % 2
                            sch.op("act", lambda a: a.activation(out=e_.t[:], in_=ps[:, bl, 0:E], func=AF.Exp, bias=s_.t[:, 1:2], scale=1.0,
                                                                 accum_out=s_.t[:, 2:3]),
                                   reads=[PB[bl], s_.b], writes=[e_.b], accw=[s_.b])

                        def a11(i):
                            s_, e_, af = R2(sm, i), R2(ex, i), R2(aff, i)
                            sch.op("dve", lambda v: v.reciprocal(out=s_.t[:, 3:4], in_=s_.t[:, 2:3]), reads=[s_.b, e_.b], accw=[s_.b])
                            sch.op("dve", lambda v: v.tensor_scalar_mul(out=af.t[:], in0=e_.t[:], scalar1=s_.t[:, 3:4]), reads=[e_.b, s_.b], writes=[af.b])

                        def a12(i):
                            af = R2(aff, i)
                            bl = 6 + i % 2
                            sch.op("pe", lambda pe: pe.transpose(out=ps[0:E, bl, 128:256], in_=af.t[:], identity=identf.t[:]),
                                   reads=[af.b, identf.b], writes=[PB[bl]])

                        def a13(i):
                            tok = slice(i * 128, (i + 1) * 128)
                            bl = 6 + i % 2
                            sch.op("dve", lambda v: v.tensor_copy(out=affT.t[32 * b:32 * b + E, tok], in_=ps[0:E, bl, 128:256]),
                                   reads=[PB[bl]], accw=[affT.b])

                        pipeline(NT, [a0, a1, a2_, a3, a4, a5, a6, a7, a8, a9, a10, a11, a12, a13])
                        if b == 0 and stop == "A2":
                            dump("affT", affT)
                            stop_here("A2")
                        sch.flush()

        if True:
            st = top
            work = T(nc, st, "tk_work", [48, S], F32)
            vals = T(nc, st, "tk_vals", [48, CAP], F32)
            idxu = T(nc, st, "tk_idxu", [48, CAP], U32)
            idxf = T(nc, st, "tk_idxf", [48, CAP], F32)
            NR = CAP // 8
            valsb = [Buf("tkv%d" % r) for r in range(NR)]
            pp = [affT, work]

            def tk_mi(r):
                sl = slice(r * 8, (r + 1) * 8)
                s_ = pp[r % 2]
                sch.op("dve", lambda v: v.max_index(out=idxu.t[:, sl], in_max=vals.t[:, sl], in_values=s_.t[:]),
                       reads=[s_.b, valsb[r]], accw=[idxu.b])
            for r in range(NR):
                sl = slice(r * 8, (r + 1) * 8)
                s_ = pp[r % 2]
                sch.op("dve", lambda v, sl=sl, s_=s_: v.max(out=vals.t[:, sl], in_=s_.t[:]), reads=[s_.b], writes=[valsb[r]])
                if r > 0:
                    tk_mi(r - 1)
                if r < NR - 1:
                    d_ = pp[(r + 1) % 2]
                    sch.op("dve", lambda v, sl=sl, s_=s_, d_=d_: v.match_replace(out=d_.t[:], in_to_replace=vals.t[:, sl], in_values=s_.t[:], imm_value=-1.0),
                           reads=[s_.b, valsb[r]], writes=[d_.b])
            tk_mi(NR - 1)
            sch.op("dve", lambda v: v.tensor_copy(out=idxf.t[:], in_=idxu.t[:]), reads=[idxu.b], writes=[idxf.b])
            sch.op("dve", lambda v: v.tensor_scalar_add(out=idxf.t[32:48, :], in0=idxf.t[32:48, :], scalar1=float(S)), reads=[idxf.b], accw=[idxf.b])
            for ct in range(2):
                bk = nbank()
                sch.op("pe", lambda pe, ct=ct, bk=bk: pe.transpose(out=ps[:, bk, 0:48], in_=vals.t[:, ct * 128:(ct + 1) * 128], identity=identf.t[0:48, 0:48]),
                       reads=valsb + [identf.b], writes=[PB[bk]])
                sch.op("dve", lambda v, ct=ct, bk=bk: v.tensor_copy(out=gT.t[:, ct, :], in_=ps[:, bk, 0:48]), reads=[PB[bk]], accw=[gT.b])
                bk2 = nbank()
                sch.op("pe", lambda pe, ct=ct, bk2=bk2: pe.transpose(out=ps[:, bk2, 0:48], in_=idxf.t[:, ct * 128:(ct + 1) * 128], identity=identf.t[0:48, 0:48]),
                       reads=[idxf.b, identf.b], writes=[PB[bk2]])
                sch.op("dve", lambda v, ct=ct, bk2=bk2: v.tensor_copy(out=idxT.t[:, ct, :], in_=ps[:, bk2, 0:48]), reads=[PB[bk2]], accw=[idxT.b])
            if stop == "T":
                dump("affT", affT); dump("gT", gT); dump("idxT", idxT); dump("vals", vals); dump("idxu", idxu)
                stop_here("T")

        with contextlib.ExitStack() as sb_:
            ring = [T(nc, sb_, "ring%d" % i, [128, 4096], BF16) for i in range(NSLOT)]
            wdt = [[T(nc, sb_, "wd%d_%d" % (hf, i), [128, 4, 512], BF16) for i in range(6)] for hf in range(2)]
            xg = [T(nc, sb_, "xg%d" % i, [128, 4, D], BF16) for i in range(2)]
            xgT = [T(nc, sb_, "xgT%d" % i, [128, 8, 512], BF16) for i in range(2)]
            hT = T(nc, sb_, "hT", [128, NFC, 512], BF16)
            sg = [T(nc, sb_, "sg%d" % i, [128, 512], F32) for i in range(2)]
            gy = [T(nc, sb_, "gy%d" % i, [128, D], F32) for i in range(4)]

            loads = []
            for e in range(E):
                for kind, fb in ([(k, f) for f in range(5) for k in ("g", "u")] + [("d0", f) for f in range(6)] + [("g", 5), ("u", 5)]
                                 + [("d1", f) for f in range(6)]):
                    loads.append((kind, e, fb))
            ring_use = [0]
            ring_done = [0]
            p2_done = [-1]
            p2a_done = [-1]
            ring_of = {}

            def issue_load(ld):
                kind, e, fb = ld
                nf = 512 if fb < 5 else 256
                if kind in ("d0", "d1"):
                    nch = nf // 128
                    hf = int(kind[1])
                    dst = wdt[hf][fb]
                    sch.dma("sp", lambda q, dst=dst, e=e, fb=fb, nch=nch, hf=hf: q.dma_start(
                        out=dst.t[:, 0:nch, :],
                        in_=wdb_d[e, fb * 512:fb * 512 + nch * 128, hf * 512:(hf + 1) * 512].rearrange("(c p) n -> p c n", p=128)),
                        "wd%d_%d" % (hf, fb), reads=[wdb_bufs[e]], writes=[dst.b])
                else:
                    si = ring_use[0] % NSLOT
                    slot = ring[si]
                    ring_of[(kind, e, fb)] = slot
                    ring_use[0] += 1
                    srcw = wg_d if kind == "g" else wu_d
                    sch.dma("pool", lambda q, slot=slot, e=e, fb=fb, nf=nf, srcw=srcw: q.dma_start(
                        out=slot.t[:, 0:8 * nf].rearrange("p (k f) -> p k f", k=8),
                        in_=srcw[e, :, fb * 512:fb * 512 + nf].rearrange("(k p) f -> p k f", p=128)),
                        "ring%d" % si, writes=[slot.b])

            lptr = [0]

            def pump():
                while lptr[0] < len(loads):
                    kind, e, fb = loads[lptr[0]]
                    if kind == "d0":
                        if e - 1 > p2a_done[0]:
                            break
                    elif kind == "d1":
                        if e - 1 > p2_done[0]:
                            break
                    else:
                        if ring_use[0] - ring_done[0] >= NSLOT:
                            break
                    issue_load(loads[lptr[0]])
                    lptr[0] += 1

            def gather(e):
                xb = xg[e % 2]
                for b in range(SPC):
                    for ct in range(2):
                        col = 32 * b + e
                        sch.dma("pool", lambda q, xb=xb, b=b, ct=ct, col=col: q.indirect_dma_start(
                            out=xb.t[:, b * 2 + ct, :], out_offset=None, in_=x1b_d[:, :],
                            in_offset=bass.IndirectOffsetOnAxis(ap=idxT.t[:, ct, col:col + 1], axis=0)),
                            "xg%d_%d" % (e % 2, b * 2 + ct), reads=[x1b_buf, idxT.b], accw=[xb.b])

            def transpose_xg(e):
                xb = xg[e % 2]
                xt = xgT[e % 2]
                for cc in range(4):
                    bk = nbank()

                    def f(pe, xb=xb, cc=cc, bk=bk):
                        pv = ps[:, bk, :].bitcast(BF16)
                        for k in range(8):
                            ins = pe.transpose(out=pv[:, k * 128:(k + 1) * 128], in_=xb.t[:, cc, k * 128:(k + 1) * 128], identity=identb)
                        return ins
                    sch.op("pe", f, reads=[xb.b, cb.b], writes=[PB[bk]])
                    eng = "act" if cc % 2 == 0 else "dve"
                    src = ps[:, bk, :].bitcast(BF16).rearrange("p (k c) -> p k c", k=8)
                    dst = xt.t[:, :, cc * 128:(cc + 1) * 128]
                    if eng == "act":
                        sch.op("act", lambda a, dst=dst, src=src: a.copy(out=dst, in_=src), reads=[PB[bk]], accw=[xt.b])
                    else:
                        sch.op("dve", lambda v, dst=dst, src=src: v.tensor_copy(out=dst, in_=src), reads=[PB[bk]], accw=[xt.b])

            pump()
            gather(0)
            transpose_xg(0)
            fctr = 0
            for e in range(E):
                xt = xgT[e % 2]
                if e + 1 < E:
                    gather(e + 1)
                for fc in range(NFC):
                    fb, jf = fc // 4, fc % 4
                    nf = 512 if fb < 5 else 256
                    pump()
                    assert ("u", e, fb) in ring_of, (e, fb, lptr[0])
                    sg_slot = ring_of[("g", e, fb)]
                    su_slot = ring_of[("u", e, fb)]
                    bG, bU = nbank(), nbank()

                    def gf(pe, slot=sg_slot, bk=bG, jf=jf, nf=nf, xt=xt):
                        wv = slot.t[:, 0:8 * nf].rearrange("p (k f) -> p k f", k=8)
                        for k in range(8):
                            ins = pe.matmul(ps[:, bk, :], lhsT=wv[:, k, jf * 128:(jf + 1) * 128], rhs=xt.t[:, k, :], start=(k == 0), stop=(k == 7))
                        return ins
                    sch.op("pe", gf, reads=[sg_slot.b, xt.b], writes=[PB[bG]])
                    sch.op("pe", lambda pe, slot=su_slot, bk=bU, jf=jf, nf=nf, xt=xt: gf(pe, slot, bk, jf, nf, xt), reads=[su_slot.b, xt.b], writes=[PB[bU]])
                    j = fctr % 2
                    fctr += 1
                    sch.op("act", lambda a, j=j, bG=bG: a.activation(out=sg[j].t[:], in_=ps[:, bG, :], func=AF.Silu), reads=[PB[bG]], writes=[sg[j].b])
                    sch.op("dve", lambda v, j=j, bU=bU, fc=fc: v.tensor_tensor(out=hT.t[:, fc, :], in0=ps[:, bU, :], in1=sg[j].t[:], op=ALU.mult),
                           reads=[PB[bU], sg[j].b], accw=[hT.b])
                    if jf == 3 or fc == NFC - 1:
                        ring_done[0] += 2
                        pump()
                if e + 1 < E:
                    transpose_xg(e + 1)
                for hf in range(2):
                    for b in range(SPC):
                        for ct in range(2):
                            cc = b * 2 + ct
                            col = 32 * b + e
                            gyt = gy[cc]
                            bk = nbank()

                            def df(pe, cc=cc, hf=hf, bk=bk):
                                for fc in range(NFC):
                                    ins = pe.matmul(ps[:, bk, :], lhsT=hT.t[:, fc, cc * 128:(cc + 1) * 128],
                                                    rhs=wdt[hf][fc // 4].t[:, fc % 4, :], start=(fc == 0), stop=(fc == NFC - 1))
                                return ins
                            sch.op("pe", df, reads=[hT.b] + [w.b for w in wdt[hf]], writes=[PB[bk]])
                            if cc % 2 == 0:
                                sch.op("act", lambda a, gyt=gyt, bk=bk, ct=ct, col=col, hf=hf: a.activation(
                                    out=gyt.t[:, hf * 512:(hf + 1) * 512], in_=ps[:, bk, :], func=AF.Copy, scale=gT.t[:, ct, col:col + 1]),
                                    reads=[PB[bk], gT.b], writes=[gyt.b] if hf == 0 else (), accw=() if hf == 0 else [gyt.b])
                            else:
                                sch.op("dve", lambda v, gyt=gyt, bk=bk, ct=ct, col=col, hf=hf: v.tensor_scalar_mul(
                                    out=gyt.t[:, hf * 512:(hf + 1) * 512], in0=ps[:, bk, :], scalar1=gT.t[:, ct, col:col + 1]),
                                    reads=[PB[bk], gT.b], writes=[gyt.b] if hf == 0 else (), accw=() if hf == 0 else [gyt.b])
                            if hf == 1:
                                sch.dma("pool", lambda q, gyt=gyt, ct=ct, col=col: q.indirect_dma_start(
                                    out=acc_d[:, :], out_offset=bass.IndirectOffsetOnAxis(ap=idxT.t[:, ct, col:col + 1], axis=0),
                                    in_=gyt.t[:, :], in_offset=None, compute_op=ALU.add),
                                    "scat%d" % cc, reads=[gyt.b, idxT.b], writes=[acc_bufs[b]])
                    if hf == 0:
                        p2a_done[0] = e
                        pump()
                p2_done[0] = e
                pump()
            if stop == "B":
                stop_here("B")
            sch.flush()

        with contextlib.ExitStack() as sc:
            def rt(name, shape, dtype, depth):
                return [T(nc, sc, "%s%d" % (name, i), shape, dtype) for i in range(depth)]
            wpg = T(nc, sc, "wpg", [128, 8, D], BF16)
            wpp = T(nc, sc, "wpp", [128, 2, D], BF16)
            lnt = [T(nc, sc, "lnt%d" % i, [128, D], F32) for i in range(4)]
            acc_t = rt("acc_t", [128, D], F32, 5)
            pin = rt("pin", [128, PLE], F32, 2)
            stc = rt("stc", [128, 16], F32, 4)
            pT = rt("pT", [128, 2, 128], BF16, 8)
            tmp = rt("tmpc", [128, D], F32, 2)
            x2 = rt("x2_", [128, D], F32, 7)
            x2b = rt("x2b", [128, D], BF16, 2)
            x2T = rt("x2T", [128, 8, 128], BF16, 2)
            sgm = rt("sgm", [128, D], F32, 2)
            r3 = rt("r3_", [128, D], F32, 4)
            std = rt("std", [128, 16], F32, 4)
            tmp3 = rt("tmp3c", [128, D], F32, 3)
            yo = rt("yo", [128, D], F32, 2)

            def R(lst, n):
                return lst[n % len(lst)]

            cast_load(wpg, wpg.t[:], wpg_d.rearrange("(k p) n -> p k n", p=128), "w0")
            cast_load(wpp, wpp.t[:], wpp_d.rearrange("(k p) n -> p k n", p=128), "w1")
            for i in range(4):
                sch.dma("sp", lambda q, i=i: q.dma_start(out=lnt[i].t[:], in_=lnp_d[2 + i]), "ln%d" % i, writes=[lnt[i].b])

            def st_a(src_t, stt):
                def f(v):
                    v.bn_stats(out=stt.t[:, 0:6], in_=src_t.t[:, 0:512])
                    return v.bn_stats(out=stt.t[:, 6:12], in_=src_t.t[:, 512:1024])
                sch.op("dve", f, reads=[src_t.b], writes=[stt.b])
                sch.op("dve", lambda v: v.bn_aggr(out=stt.t[:, 12:14], in_=stt.t[:, 0:12]), reads=[stt.b], accw=[stt.b])

            def st_b(stt, ec=1):
                sch.op("act", lambda a: a.activation(out=stt.t[:, 14:15], in_=stt.t[:, 13:14], func=AF.Sqrt, bias=epsr.t[:, ec:ec + 1], scale=1.0),
                       reads=[stt.b, epsr.b], accw=[stt.b])

            def st_c(stt):
                sch.op("dve", lambda v: v.reciprocal(out=stt.t[:, 14:15], in_=stt.t[:, 14:15]), reads=[stt.b], accw=[stt.b])
                sch.op("dve", lambda v: v.scalar_tensor_tensor(out=stt.t[:, 15:16], in0=stt.t[:, 12:13], scalar=-1.0, in1=stt.t[:, 14:15],
                                                               op0=ALU.mult, op1=ALU.mult), reads=[stt.b], accw=[stt.b])

            def c0(n):
                b, i = divmod(n, NT)
                tok = slice(i * 128, (i + 1) * 128)
                row0 = b * S + i * 128
                at, pi = R(acc_t, n), R(pin, n)
                sch.dma("sp", lambda q: q.dma_start(out=at.t[:], in_=acc_d[row0:row0 + 128, :]), "acct%d" % (n % len(acc_t)),
                        reads=[acc_bufs[b]], writes=[at.b])
                sch.dma("sp", lambda q: q.dma_start(out=pi.t[:], in_=p_d[b, tok, :]), "pin%d" % (n % len(pin)), writes=[pi.b])

            def c1(n):
                st_a(R(acc_t, n), R(stc, n))
                bp = n % 2
                pi = R(pin, n)

                def ptf(pe):
                    for k in range(2):
                        ins = pe.transpose(out=ps[:, bp, k * 128:(k + 1) * 128], in_=pi.t[:, k * 128:(k + 1) * 128], identity=identf.t[:])
                    return ins
                sch.op("pe", ptf, reads=[pi.b, identf.b], writes=[PB[bp]])

            def c2(n):
                st_b(R(stc, n))
                bp = n % 2
                pt_ = R(pT, n)
                sch.op("act", lambda a: a.copy(out=pt_.t[:].rearrange("p k c -> p (k c)"), in_=ps[:, bp, 0:256]),
                       reads=[PB[bp]], writes=[pt_.b])

            def c3(n):
                st_c(R(stc, n))

            def c4(n):
                at, stt, tm = R(acc_t, n), R(stc, n), R(tmp, n)
                sch.op("act", lambda a: a.activation(out=tm.t[:], in_=at.t[:], func=AF.Identity, bias=stt.t[:, 15:16], scale=stt.t[:, 14:15]),
                       reads=[at.b, stt.b], writes=[tm.b])

            def c5(n):
                tm, xx = R(tmp, n), R(x2, n)
                sch.op("pool", lambda g: g.tensor_tensor(out=tm.t[:], in0=tm.t[:], in1=lnt[0].t[:], op=ALU.mult), reads=[tm.b, lnt[0].b], writes=[tm.b])
                sch.op("pool", lambda g: g.tensor_tensor(out=xx.t[:], in0=tm.t[:], in1=lnt[1].t[:], op=ALU.add), reads=[tm.b, lnt[1].b], writes=[xx.b])

            def c6(n):
                xx, xb = R(x2, n), R(x2b, n)
                sch.op("act", lambda a: a.copy(out=xb.t[:], in_=xx.t[:]), reads=[xx.b], writes=[xb.b])

            def c7(n):
                xb = R(x2b, n)
                bk = 2 + n % 2

                def trf(pe):
                    pv = ps[:, bk, :].bitcast(BF16)
                    for k in range(8):
                        ins = pe.transpose(out=pv[:, k * 128:(k + 1) * 128], in_=xb.t[:, k * 128:(k + 1) * 128], identity=identb)
                    return ins
                sch.op("pe", trf, reads=[xb.b, cb.b], writes=[PB[bk]])

            def c8(n):
                bk = 2 + n % 2
                xt_ = R(x2T, n)
                sch.op("act", lambda a: a.copy(out=xt_.t[:].rearrange("p k c -> p (k c)"), in_=ps[:, bk, :].bitcast(BF16)),
                       reads=[PB[bk]], writes=[xt_.b])

            def c9(n):
                xt_ = R(x2T, n)

                def gf(pe):
                    for hf in range(2):
                        for k in range(8):
                            ins = pe.matmul(ps[:, 4 + hf, :], lhsT=xt_.t[:, k, :], rhs=wpg.t[:, k, hf * 512:(hf + 1) * 512],
                                            start=(k == 0), stop=(k == 7))
                    return ins
                sch.op("pe", gf, reads=[xt_.b, wpg.b], writes=[PB[4], PB[5]])

            def c10(n):
                pt_, sg_ = R(pT, n), R(sgm, n)
                sch.op("act", lambda a: a.activation(out=sg_.t[:], in_=ps2(4), func=AF.Sigmoid), reads=[PB[4], PB[5]], writes=[sg_.b])

                def ef(pe):
                    for hf in range(2):
                        for k in range(2):
                            ins = pe.matmul(ps[:, 6 + hf, :], lhsT=pt_.t[:, k, :], rhs=wpp.t[:, k, hf * 512:(hf + 1) * 512],
                                            start=(k == 0), stop=(k == 1))
                    return ins
                sch.op("pe", ef, reads=[pt_.b, wpp.b], writes=[PB[6], PB[7]])

            def c11(n):
                sg_, xx, rr_, sd = R(sgm, n), R(x2, n), R(r3, n), R(std, n)
                sch.op("dve", lambda v: v.scalar_tensor_tensor(out=sg_.t[:], in0=ps2(6), scalar=1.0 / ALPHA, in1=sg_.t[:],
                                                               op0=ALU.mult, op1=ALU.mult),
                       reads=[PB[6], PB[7], sg_.b], writes=[sg_.b])
                sch.op("dve", lambda v: v.tensor_tensor(out=rr_.t[:], in0=xx.t[:], in1=sg_.t[:], op=ALU.add), reads=[xx.b, sg_.b], writes=[rr_.b])
                st_a(rr_, sd)

            def c12(n):
                st_b(R(std, n), ec=2)

            def c13(n):
                st_c(R(std, n))

            def c14(n):
                rr_, sd, t3_ = R(r3, n), R(std, n), R(tmp3, n)
                sch.op("act", lambda a: a.activation(out=t3_.t[:], in_=rr_.t[:], func=AF.Identity, bias=sd.t[:, 15:16], scale=sd.t[:, 14:15]),
                       reads=[rr_.b, sd.b], writes=[t3_.b])

            def c15(n):
                t3_ = R(tmp3, n)
                sch.op("dve", lambda v: v.tensor_tensor(out=t3_.t[:], in0=t3_.t[:], in1=lnt[2].t[:], op=ALU.mult), reads=[t3_.b, lnt[2].b], writes=[t3_.b])

            def c16(n):
                b, i = divmod(n, NT)
                tok = slice(i * 128, (i + 1) * 128)
                t3_, yy = R(tmp3, n), R(yo, n)
                sch.op("pool", lambda g: g.tensor_tensor(out=yy.t[:], in0=t3_.t[:], in1=lnt[3].t[:], op=ALU.add), reads=[t3_.b, lnt[3].b], writes=[yy.b])
                sch.dma("sp", lambda q: q.dma_start(out=out_d[b, tok, :], in_=yy.t[:]), "outst%d" % (n % len(yo)), reads=[yy.b])

            pipeline(SPC * NT, [c0, c1, c2, c3, c4, c5, c6, c7, c8, c9, c10, c11, c12, c13, c14, c15, c16])
            sch.flush()

    except StopBuild:
        pass
    nc._dumps = dumps
    return nc


def _rope_tables():
    t = np.arange(S)
    row = (t // 64).astype(np.float32)
    col = (t % 64).astype(np.float32)
    tabs = np.zeros((4, 128, S), np.float32)
    inv16 = (10000.0 ** (-np.arange(16, dtype=np.float32) * 2.0 / 32.0)).astype(np.float32)
    for p in range(128):
        d = p % 64
        blk, f = d // 16, d % 16
        pos = row if blk < 2 else col
        ang = (pos * inv16[f]).astype(np.float32)
        tabs[0, p] = np.cos(ang)
        tabs[1, p] = np.sin(ang) * (-1.0 if blk in (0, 2) else 1.0)
    tabs[2, :, :] = 1.0
    inv8 = (10000.0 ** (-np.arange(8, dtype=np.float32) * 2.0 / 16.0)).astype(np.float32)
    for p in range(64, 96):
        d = p - 64
        blk, f = d // 8, d % 8
        pos = row if blk < 2 else col
        ang = (pos * inv8[f]).astype(np.float32)
        tabs[2, p] = np.cos(ang)
        tabs[3, p] = np.sin(ang) * (-1.0 if blk in (0, 2) else 1.0)
    tabs[3, 96:128] = tabs[3, 64:96]
    return tabs


_PERM64 = np.concatenate([np.arange(16, 32), np.arange(0, 16), np.arange(48, 64), np.arange(32, 48)])
_PERM32 = np.concatenate([np.arange(8, 16), np.arange(0, 8), np.arange(24, 32), np.arange(16, 24)])


def _prep_shared(inp):
    f32 = np.float32
    w_in = np.asarray(inp["w_in"][0], f32)
    qcols = []
    for j in range(4):
        qcols += list(range(j * 64, (j + 1) * 64)) + list(range((j + 4) * 64, (j + 5) * 64))
    cols = np.array(qcols + list(range(512, 1312)))
    w_in_p = np.ascontiguousarray(w_in[:, cols])
    rot_cols = []
    for hblk in range(10):
        rot_cols += list(hblk * 64 + _PERM64)
    w_in_rot = np.ascontiguousarray(np.concatenate([w_in_p[:, np.array(rot_cols)], w_in[:, 1280 + _PERM32]], axis=1))
    w_uq = np.asarray(inp["w_uq"][0], f32)
    rc = []
    for h in range(8):
        rc += list(range(h * 96, (h + 1) * 96)) + list(h * 96 + 64 + _PERM32)
    w_uq_cat = np.ascontiguousarray(w_uq[:, np.array(rc)])
    w_out = np.asarray(inp["w_out"][0], f32)
    rows = []
    for j in range(4):
        rows += list(range(j * 64, (j + 1) * 64)) + list(range((j + 4) * 64, (j + 5) * 64))
    rows += list(range(512, 1024))
    w_out_p = np.ascontiguousarray(w_out[np.array(rows), :])
    qn = np.asarray(inp["q_norm"][0], f32)
    kn = np.asarray(inp["k_norm"][0], f32)
    vecs = np.zeros((128, 8), f32)
    vecs[:, 0] = np.tile(qn, 2)
    vecs[:, 1] = np.tile(qn[_PERM64], 2)
    vecs[:, 2] = np.tile(kn, 2)
    vecs[:, 3] = np.tile(kn[_PERM64], 2)
    vecs[:, 4:6] = np.asarray(inp["cq_norm"][0], f32).reshape(2, 128).T
    vecs[:, 6:8] = np.asarray(inp["ckv_norm"][0], f32).reshape(2, 128).T
    lnp = np.stack([np.broadcast_to(np.asarray(inp[k][0], f32)[None, :], (128, D))
                    for k in ("ln_attn_g", "ln_attn_b", "ln_ffn_g", "ln_ffn_b", "ln_ple_g", "ln_ple_b")]).astype(f32)
    ident = np.eye(128, dtype=f32)
    cbm = np.zeros((128, 3, 128), f32)
    cbm[:, 0, :] = ident
    cbm[0:64, 1, 0:64] = 1.0 / 64
    cbm[64:128, 1, 64:128] = 1.0 / 64
    cbm[:, 2, :] = 1.0 / 256
    return {
        "w_in_p": w_in_p, "w_in_rot": w_in_rot, "w_uq_cat": w_uq_cat,
        "w_ukv": np.ascontiguousarray(np.asarray(inp["w_ukv"][0], f32)), "w_out_p": w_out_p,
        "w_router": np.ascontiguousarray(np.asarray(inp["w_router"][0], f32)),
        **({k: np.ascontiguousarray(np.asarray(inp[k][0], f32)) for k in ("w_gate", "w_up", "w_down") if k in inp}),
        "w_ple_proj": np.ascontiguousarray(np.asarray(inp["w_ple_proj"][0], f32)),
        "w_ple_gate": np.ascontiguousarray(np.asarray(inp["w_ple_gate"][0], f32)),
        "lnp": np.ascontiguousarray(lnp), "vecs": vecs, "tabs": _rope_tables(),
        "ident_f": ident, "cb": cbm.astype(ml_dtypes.bfloat16),
    }


_NC_CACHE = {}


def kernel(**inputs):
    shared = _prep_shared(inputs)
    x = np.asarray(inputs["x"], np.float32)
    p = np.asarray(inputs["p"], np.float32)[0]
    if "nc" not in _NC_CACHE:
        _NC_CACHE["nc"] = build_program()
    nc = _NC_CACHE["nc"]
    in_maps = []
    for c in range(NCORES):
        m = dict(shared)
        m["x"] = np.ascontiguousarray(x[c * SPC:(c + 1) * SPC])
        m["p"] = np.ascontiguousarray(p[c * SPC:(c + 1) * SPC])
        in_maps.append(m)
    res = run_bass_kernel_spmd(nc, in_maps, core_ids=list(range(NCORES)))
    _NC_CACHE["last"] = res
    out = np.concatenate([np.asarray(r["out"]) for r in res.results], axis=0)
    return out.astype(np.float32)
```
